# Optimizing a Trainium2 kernel written in Bass

```python
import jax, jax.numpy as jnp
from jax import lax
import numpy as np

D_MODEL = 1024
BATCH = 8
SEQ = 2048
DEPTH = 2

CHUNK = 64
N_PREV_CHUNKS = 8
BAND = (N_PREV_CHUNKS + 1) * CHUNK
BAND_PAD = N_PREV_CHUNKS * CHUNK
D_CONV = D_MODEL // 2
D_ATT = D_MODEL - D_CONV
D_MIX = D_CONV + D_ATT
HEAD_DIM = 64
N_HEADS = D_ATT // HEAD_DIM
CONV_WIDTH = 31
MAX_REL = 128
N_REL = 2 * MAX_REL + 1
D_IN_COLS = 2 * D_CONV + 3 * D_ATT
D_FF_DENSE = 2816
N_EXPERTS = 8
TOP_K = 2
D_FF_EXPERT = 3584
N_DENSE = (DEPTH + 1) // 2
N_MOE = DEPTH // 2
EPS = 1e-6
NEG_INF = -1e30

kernel_name = "hybrid_conformer_chunkattn_moe_adaln"


def rmsnorm(x, g):
    xf = x.astype(jnp.float32)
    y = xf * lax.rsqrt(jnp.mean(xf * xf, axis=-1, keepdims=True) + EPS)
    return (y * g.astype(jnp.float32)).astype(x.dtype)


def layernorm(x, g, b):
    xf = x.astype(jnp.float32)
    mu = jnp.mean(xf, axis=-1, keepdims=True)
    xc = xf - mu
    var = jnp.mean(xc * xc, axis=-1, keepdims=True)
    y = xc * lax.rsqrt(var + EPS) * g.astype(jnp.float32) + b.astype(jnp.float32)
    return y.astype(x.dtype)


def modulate(h, shift, scale):
    return h * (1 + scale[:, None, :]) + shift[:, None, :]


def conformer_conv(u, conv_w, conv_b, ln_g, ln_b):
    a, g = jnp.split(u, 2, axis=-1)
    z = a * jax.nn.sigmoid(g)
    z = lax.conv_general_dilated(
        z, conv_w, window_strides=(1,), padding=[(CONV_WIDTH - 1, 0)],
        dimension_numbers=("NWC", "WIO", "NWC"),
        feature_group_count=D_CONV) + conv_b
    z = layernorm(z, ln_g, ln_b)
    return jax.nn.silu(z)


def chunk_band_attention(q, k, v, q_norm_g, k_norm_g, rel_bias):
    B, S = q.shape[0], q.shape[1]
    nc = S // CHUNK
    q = rmsnorm(q.reshape(B, S, N_HEADS, HEAD_DIM), q_norm_g)
    k = rmsnorm(k.reshape(B, S, N_HEADS, HEAD_DIM), k_norm_g)
    v = v.reshape(B, S, N_HEADS, HEAD_DIM)
    pad = ((0, 0), (BAND_PAD, 0), (0, 0), (0, 0))
    kp = jnp.pad(k, pad)
    vp = jnp.pad(v, pad)
    idx = jnp.arange(nc)[:, None] * CHUNK + jnp.arange(BAND)[None, :]
    kb = kp[:, idx]
    vb = vp[:, idx]
    qc = q.reshape(B, nc, CHUNK, N_HEADS, HEAD_DIM)
    s = jnp.einsum("bnqhd,bnkhd->bnhqk", qc, kb).astype(jnp.float32) * (HEAD_DIM ** -0.5)
    rel = jnp.arange(BAND)[None, :] - BAND_PAD - jnp.arange(CHUNK)[:, None]
    rel_idx = jnp.clip(rel, -MAX_REL, MAX_REL) + MAX_REL
    bias = rel_bias.astype(jnp.float32)[:, rel_idx]
    valid = idx >= BAND_PAD
    s = jnp.where(valid[None, :, None, None, :], s + bias[None, None], NEG_INF)
    p = jax.nn.softmax(s, axis=-1).astype(v.dtype)
    o = jnp.einsum("bnhqk,bnkhd->bnqhd", p, vb)
    return o.reshape(B, S, D_ATT)


def swiglu(h, w_gate, w_up, w_down):
    return (jax.nn.silu(h @ w_gate) * (h @ w_up)) @ w_down


def moe_swiglu(h, w_router, b_router, w_gate, w_up, w_down):
    B, S, D = h.shape
    t = h.reshape(B * S, D)
    logits = (t @ w_router).astype(jnp.float32) + b_router.astype(jnp.float32)
    top_val, top_idx = lax.top_k(logits, TOP_K)
    top_w = jax.nn.softmax(top_val, axis=-1)
    combine = jnp.sum(jax.nn.one_hot(top_idx, N_EXPERTS, dtype=jnp.float32)
                      * top_w[..., None], axis=1).astype(t.dtype)
    out = jnp.zeros_like(t)
    for e in range(N_EXPERTS):
        out = out + combine[:, e:e + 1] * swiglu(t, w_gate[e], w_up[e], w_down[e])
    return out.reshape(B, S, D)


def setup_inputs(seed: int = 0) -> dict:
    key = jax.random.key(seed)
    ks = jax.random.split(key, 24)
    f32 = jnp.float32

    def nrm(k, shape, scale):
        return jax.random.normal(k, shape, f32) * scale

    return {
        "x": nrm(ks[0], (BATCH, SEQ, D_MODEL), 1.0),
        "c": nrm(ks[1], (BATCH, D_MODEL), 1.0),
        "w_ada": nrm(ks[2], (DEPTH, D_MODEL, 6 * D_MODEL), 0.5 * D_MODEL ** -0.5),
        "b_ada": nrm(ks[3], (DEPTH, 6 * D_MODEL), 0.02),
        "norm_mix_g": 1.0 + nrm(ks[4], (DEPTH, D_MODEL), 0.02),
        "norm_ffn_g": 1.0 + nrm(ks[5], (DEPTH, D_MODEL), 0.02),
        "w_in": nrm(ks[6], (DEPTH, D_MODEL, D_IN_COLS), D_MODEL ** -0.5),
        "w_out": nrm(ks[7], (DEPTH, D_MIX, D_MODEL), D_MIX ** -0.5),
        "conv_w": nrm(ks[8], (DEPTH, CONV_WIDTH, 1, D_CONV), CONV_WIDTH ** -0.5),
        "conv_b": nrm(ks[9], (DEPTH, D_CONV), 0.02),
        "conv_ln_g": 1.0 + nrm(ks[10], (DEPTH, D_CONV), 0.02),
        "conv_ln_b": nrm(ks[11], (DEPTH, D_CONV), 0.02),
        "q_norm_g": 1.0 + nrm(ks[12], (DEPTH, HEAD_DIM), 0.02),
        "k_norm_g": 1.0 + nrm(ks[13], (DEPTH, HEAD_DIM), 0.02),
        "rel_bias": nrm(ks[14], (DEPTH, N_HEADS, N_REL), 0.5),
        "ffn_w_gate": nrm(ks[15], (N_DENSE, D_MODEL, D_FF_DENSE), D_MODEL ** -0.5),
        "ffn_w_up": nrm(ks[16], (N_DENSE, D_MODEL, D_FF_DENSE), D_MODEL ** -0.5),
        "ffn_w_down": nrm(ks[17], (N_DENSE, D_FF_DENSE, D_MODEL), D_FF_DENSE ** -0.5),
        "moe_w_router": nrm(ks[18], (N_MOE, D_MODEL, N_EXPERTS), D_MODEL ** -0.5),
        "moe_b_router": nrm(ks[19], (N_MOE, N_EXPERTS), 0.01),
        "moe_w_gate": nrm(ks[20], (N_MOE, N_EXPERTS, D_MODEL, D_FF_EXPERT), D_MODEL ** -0.5),
        "moe_w_up": nrm(ks[21], (N_MOE, N_EXPERTS, D_MODEL, D_FF_EXPERT), D_MODEL ** -0.5),
        "moe_w_down": nrm(ks[22], (N_MOE, N_EXPERTS, D_FF_EXPERT, D_MODEL), D_FF_EXPERT ** -0.5),
    }


def reference(x, c, w_ada, b_ada, norm_mix_g, norm_ffn_g, w_in, w_out,
              conv_w, conv_b, conv_ln_g, conv_ln_b, q_norm_g, k_norm_g, rel_bias,
              ffn_w_gate, ffn_w_up, ffn_w_down,
              moe_w_router, moe_b_router, moe_w_gate, moe_w_up, moe_w_down):
    c_act = jax.nn.silu(c)
    for l in range(DEPTH):
        mod = c_act @ w_ada[l] + b_ada[l]
        sh1, sc1, g1, sh2, sc2, g2 = jnp.split(mod, 6, axis=-1)

        h = modulate(rmsnorm(x, norm_mix_g[l]), sh1, sc1)
        proj = h @ w_in[l]
        u_conv = proj[..., :2 * D_CONV]
        q = proj[..., 2 * D_CONV:2 * D_CONV + D_ATT]
        k = proj[..., 2 * D_CONV + D_ATT:2 * D_CONV + 2 * D_ATT]
        v = proj[..., 2 * D_CONV + 2 * D_ATT:]
        y_conv = conformer_conv(u_conv, conv_w[l], conv_b[l], conv_ln_g[l], conv_ln_b[l])
        y_att = chunk_band_attention(q, k, v, q_norm_g[l], k_norm_g[l], rel_bias[l])
        y = jnp.concatenate([y_conv, y_att], axis=-1) @ w_out[l]
        x = x + g1[:, None, :] * y

        h = modulate(rmsnorm(x, norm_ffn_g[l]), sh2, sc2)
        if l % 2 == 0:
            i = l // 2
            f = swiglu(h, ffn_w_gate[i], ffn_w_up[i], ffn_w_down[i])
        else:
            i = l // 2
            f = moe_swiglu(h, moe_w_router[i], moe_b_router[i],
                           moe_w_gate[i], moe_w_up[i], moe_w_down[i])
        x = x + g2[:, None, :] * f
    return x
```

```python
import numpy as np
from contextlib import ExitStack
import concourse.bass as bass
import concourse.mybir as mybir
from concourse.bass_utils import run_bass_kernel_spmd

F32 = mybir.dt.float32
BF16 = mybir.dt.bfloat16
AF = mybir.ActivationFunctionType
ALU = mybir.AluOpType
AX = mybir.AxisListType

D = 1024
S = 2048
NT = 4
NSUB = 16
DEPTH = 2
NH = 8
FF_DENSE = 2816
FF_EXP = 3584
NE = 8
EPS = 1e-6
NSLAB = 3
MASK_NEG = -30000.0
SPARSE_MOE = True


class Op:
    __slots__ = ("eng", "fn", "reads", "writes", "dma", "deps", "signal", "sem", "val", "region")

    def __init__(self, eng, fn, reads, writes, dma):
        self.eng = eng
        self.fn = fn
        self.reads = tuple(reads)
        self.writes = tuple(writes)
        self.dma = dma
        self.deps = []
        self.signal = False
        self.sem = None
        self.val = 0
        self.region = None


class Region:
    def __init__(self, flag_ap, parent=None):
        self.flag_ap = flag_ap
        self.pre = []
        self.parent = parent
        self.depth = 0 if parent is None else parent.depth + 1

    def ancestor_at(self, depth):
        r = self
        while r.depth > depth:
            r = r.parent
        return r


class Prog:
    ENGS = ("pe", "act", "dve", "pool", "sp")

    def __init__(self):
        self.ops = []
        self.cur_region = None

    def op(self, eng, fn, reads=(), writes=(), dma=None):
        o = Op(eng, fn, reads, writes, dma)
        o.region = self.cur_region
        self.ops.append(o)
        return o

    def begin_region(self, flag_ap):
        r = Region(flag_ap, self.cur_region)
        self.ops.append(("region_begin", r))
        self.cur_region = r

    def end_region(self):
        self.cur_region = self.cur_region.parent
        self.ops.append(("barrier_all",))

    def barrier(self, engs=("pe", "act", "dve")):
        self.ops.append(("barrier", tuple(engs)))

    def analyse(self):
        last_w = {}
        readers = {}
        last_dma = {}
        last_on_eng = {}
        pending = {e: [] for e in self.ENGS}
        pending_all = {e: [] for e in self.ENGS}
        real = []
        for o in self.ops:
            if isinstance(o, tuple):
                if o[0] == "barrier":
                    lasts = [last_on_eng[e] for e in o[1] if e in last_on_eng]
                    for e in o[1]:
                        pending[e] = list(lasts)
                else:
                    lasts = [last_on_eng[e] for e in self.ENGS if e in last_on_eng] + list(last_dma.values())
                    for d in lasts:
                        d.signal = True
                    if o[0] == "region_begin":
                        o[1].pre = list(lasts)
                    for e in self.ENGS:
                        pending_all[e] = list(lasts)
                continue
            deps = set()
            for r in o.reads:
                if r in last_w:
                    deps.add(last_w[r])
            for w in o.writes:
                if w in last_w:
                    deps.add(last_w[w])
                for rd in readers.get(w, ()):
                    deps.add(rd)
            if o.dma is not None and o.dma in last_dma:
                deps.add(last_dma[o.dma])
            final = []
            for d in deps:
                if d is o:
                    continue
                if d.dma is None and d.eng == o.eng:
                    if o.eng == "pe":
                        continue
                final.append(d)
            for d in pending[o.eng]:
                if d.eng != o.eng or d.dma is not None:
                    final.append(d)
            pending[o.eng] = []
            final.extend(pending_all[o.eng])
            pending_all[o.eng] = []
            for d in final:
                d.signal = True
            o.deps = final
            for w in o.writes:
                last_w[w] = o
                readers[w] = []
            for r in o.reads:
                readers.setdefault(r, []).append(o)
            if o.dma is not None:
                last_dma[o.dma] = o
            last_on_eng[o.eng] = o
            real.append(o)
        self.real = real

    def emit(self, nc, stack):
        self.analyse()
        eng_sem = {e: stack.enter_context(nc.semaphore("s_" + e)) for e in self.ENGS}
        dma_sem = {}
        cnt = {}
        for o in self.real:
            if not o.signal:
                continue
            if o.dma is not None:
                if o.dma not in dma_sem:
                    dma_sem[o.dma] = stack.enter_context(nc.semaphore("d%d" % len(dma_sem)))
                o.sem = dma_sem[o.dma]
                cnt[o.sem] = cnt.get(o.sem, 0) + 16
            else:
                o.sem = eng_sem[o.eng]
                cnt[o.sem] = cnt.get(o.sem, 0) + 1
            o.val = cnt[o.sem]
        per = {e: [o for o in self.real if o.eng == e] for e in self.ENGS}
        block = stack.enter_context(nc.Block())

        def run(h, ops):
            known = {}

            def waits(deps):
                need = {}
                for d in deps:
                    if known.get(d.sem, 0) < d.val:
                        need[d.sem] = max(need.get(d.sem, 0), d.val)
                for sem, v in need.items():
                    h.wait_ge(sem, v)
                    known[sem] = v

            def emit_op(o):
                waits(o.deps)
                if o.fn is None:
                    return
                ins = o.fn(h)
                if o.signal:
                    ins.then_inc(o.sem, 16 if o.dma is not None else 1)

            def emit_seq(seq, depth, rf):
                i = 0
                while i < len(seq):
                    o = seq[i]
                    od = -1 if o.region is None else o.region.depth
                    if od < depth:
                        emit_op(o)
                        i += 1
                        continue
                    R = o.region.ancestor_at(depth)
                    j = i
                    while j < len(seq) and seq[j].region is not None and seq[j].region.depth >= depth \
                            and seq[j].region.ancestor_at(depth) is R:
                        j += 1
                    body = seq[i:j]
                    waits(R.pre)
                    saved = dict(known)
                    h.reg_load(rf, R.flag_ap)
                    with h.If_ne(rf, 0):
                        emit_seq(body, depth + 1, rf)
                    with h.Else():
                        incs = {}
                        for b in body:
                            if b.signal:
                                incs[b.sem] = incs.get(b.sem, 0) + (16 if b.dma is not None else 1)
                        for sem, v in incs.items():
                            h.sem_inc(sem, v)
                    known.clear()
                    known.update(saved)
                    i = j

            with h.register("rflag") as rf:
                emit_seq(ops, 0, rf)

        @block.tensor
        def _(h):
            run(h, per["pe"])

        @block.scalar
        def _(h):
            run(h, per["act"])

        @block.vector
        def _(h):
            run(h, per["dve"])

        @block.gpsimd
        def _(h):
            run(h, per["pool"])

        @block.sync
        def _(h):
            run(h, per["sp"])


def build_program(layers, debug=None):
    nc = bass.Bass("TRN2", target_bir_lowering=False)
    P = Prog()
    stack = ExitStack()

    def din(name, shape, dt=F32):
        return nc.dram_tensor(name, list(shape), dt, kind="ExternalInput").ap()

    x_in = din("x", [S, D])
    cT_in = din("cT", [128, 8])
    w_ada = din("w_ada", [DEPTH, D, 6 * D])
    b_ada = din("b_ada", [DEPTH, 6 * D])
    gmix_in = din("gmix", [DEPTH, 128, 8])
    gffn_in = din("gffn", [DEPTH, 128, 8])
    w_in = din("w_in", [DEPTH, D, 2560])
    w_out = din("w_out", [DEPTH, D, D])
    convw_in = din("convw", [DEPTH, 128, 4, 31])
    convp_in = din("convp", [DEPTH, 128, 3, 4])
    qkg_in = din("qkg", [DEPTH, 128, 2])
    bias2_in = din("bias2", [DEPTH, 128, NH, 640])
    if 0 in layers:
        ffn_wg = din("ffn_wg", [1, D, FF_DENSE])
        ffn_wu = din("ffn_wu", [1, D, FF_DENSE])
        ffn_wd = din("ffn_wd", [1, FF_DENSE, D])
    wr_in = din("wr", [1, 128, 8, NE])
    br_in = din("br", [1, 128, NE])
    if 1 in layers:
        moe_wgu = din("moe_wgu", [1, NE, D, 2 * FF_EXP])
        moe_wd = din("moe_wd", [1, NE, FF_EXP, D])
    if 0 not in layers:
        ffn_wg = ffn_wu = ffn_wd = None
    y_out = nc.dram_tensor("y", [S, D], F32, kind="ExternalOutput").ap()
    dbg_out = {}
    if debug:
        for name, shape in debug.items():
            dbg_out[name] = nc.dram_tensor("dbg_" + name, list(shape), F32, kind="ExternalOutput").ap()

    def sb(name, shape, dt):
        return stack.enter_context(nc.sbuf_tensor("sb_" + name, list(shape), dt))

    x_tok = sb("x_tok", [128, NSUB, D], F32)
    kvact = sb("kvact", [128, 16384], BF16)
    kT = kvact[:, 0:8192].rearrange("p (c t) -> p c t", c=4)
    v_tok = kvact[:, 8192:16384].rearrange("p (s d) -> p s d", s=16)
    act = kvact[:, 0:28 * 512].rearrange("p (j t) -> p j t", j=28)
    hT = sb("hT", [128, 8, 512], BF16)
    qT = sb("qT", [128, 4, 512], BF16)
    zb = sb("zb", [128, 4, 542], BF16)
    ycat = sb("ycat", [128, 8, 512], BF16)
    slabs = [sb("slab%d" % i, [128, 8, 512], BF16) for i in range(NSLAB)]
    bias2 = sb("bias2", [128, NH, 640], BF16)
    gbc = sb("gbc", [128, 2, D], F32)
    arena = sb("arena", [128, 7680], F32)
    ident_f = sb("ident_f", [128, 128], F32)
    ident_b = sb("ident_b", [128, 128], BF16)
    ones_f = sb("ones_f", [128, 128], F32)
    bdiag_f = sb("bdiag_f", [128, 128], F32)
    cT = sb("cT", [128, 8], F32)
    cact = sb("cact", [128, 8], BF16)
    modcols = [sb("modcol%d" % i, [128, 4, 8], F32) for i in range(2)]
    LCUR = [0]
    gcol = sb("gcol", [128, 2, 8], F32)
    ABcols = [sb("ABcol%d" % i, [128, 2, 8], F32) for i in range(2)]
    convw = sb("convw", [128, 4, 31], F32)
    convp = sb("convp", [128, 3, 4], F32)
    qkg = sb("qkg", [128, 2], F32)
    wr = sb("wr", [128, 8, NE], F32)
    brb = sb("brb", [128, NE], F32)
    comb = sb("comb", [128, NSUB, NE], F32)
    rowb = sb("rowb", [1, 2, 512], F32)
    brow = sb("brow", [1, 2, 512], F32)
    small = sb("small", [128, 96], F32)
    one_f = ones_f[0:1, 0:1]
    epsc = sb("epsc", [128, 1], F32)

    psd = [stack.enter_context(nc.psum_tensor("psd%d" % i, [128, 1024], F32)) for i in range(4)]
    ps = [psd[i // 2][:, (i % 2) * 512:(i % 2) * 512 + 512] for i in range(8)]

    misc_i = [0]

    def dma_sp(out, in_, writes, reads=()):
        lane = ("misc", misc_i[0] % 4)
        misc_i[0] += 1
        P.op("sp", lambda h: h.dma_start(out=out, in_=in_), reads=reads, writes=writes, dma=lane)

    slab_i = [0]

    def load_slab(src_ap, kparts, ncols):
        i = slab_i[0] % NSLAB
        slab_i[0] += 1
        t = slabs[i]
        key = ("slab", i)
        P.op("pool", lambda h: h.dma_start(out=t[:, 0:kparts, 0:ncols], in_=src_ap),
             writes=[key], dma=key)
        return t, key

    def mm(out, lhsT, rhs, start, stop, reads, writes):
        P.op("pe", lambda h: h.matmul(out, lhsT, rhs, start=start, stop=stop), reads=reads, writes=writes)

    def dve(fn, reads, writes):
        P.op("dve", fn, reads=reads, writes=writes)

    def actop(fn, reads, writes):
        P.op("act", fn, reads=reads, writes=writes)

    def A(off, n):
        return arena[:, off:off + n]

    def AB(off_f32, n_bf16):
        return arena[:, off_f32:off_f32 + n_bf16 // 2].bitcast(BF16)

    dve(lambda h: h.memset(ones_f[:], 1.0), [], ["ones_f"])
    dve(lambda h: h.memset(epsc[:], EPS), [], ["epsc"])
    dve(lambda h: h.memset(bdiag_f[:], 0.0), [], ["bdiag_f"])
    dve(lambda h: h.memset(bdiag_f[0:64, 0:64], 1.0), [], ["bdiag_f"])
    dve(lambda h: h.memset(bdiag_f[64:128, 64:128], 1.0), [], ["bdiag_f"])
    dve(lambda h: h.memset(zb[:, :, 0:30], 0.0), [], ["zb"])
    ident_in = din("ident", [128, 128])
    I32 = mybir.dt.int32
    ustrict = sb("ustrict", [128, 128], F32)
    iota_f = sb("iota_f", [128, 512], F32)
    ustrict_in = din("ustrict", [128, 128])
    iota_in = din("iota", [128, 512])
    dma_sp(ustrict[:], ustrict_in[:, :], ["ustrict"])
    dma_sp(iota_f[:], iota_in[:, :], ["iota_f"])
    dma_sp(ident_f[:], ident_in[:, :], ["ident_f"])
    dve(lambda h: h.tensor_copy(out=ident_b[:], in_=ident_f[:]), ["ident_f"], ["ident_b"])
    dma_sp(cT[:], cT_in[:, :], ["cT"])
    actop(lambda h: h.activation(out=cact[:], in_=cT[:], func=AF.Silu), ["cT"], ["cact"])
    xv = x_in.rearrange("(s p) d -> p s d", p=128)
    for s4 in range(4):
        P.op("sp", lambda h, s4=s4: h.dma_start(out=x_tok[:, 4 * s4:4 * s4 + 4, :], in_=xv[:, 4 * s4:4 * s4 + 4, :]),
             writes=[("x", 4 * s4 + i) for i in range(4)], dma=("xio", s4))
    P.barrier()

    def layer_consts(l):
        dma_sp(gcol[:, 0, :], gmix_in[l], ["gcol"])
        dma_sp(gcol[:, 1, :], gffn_in[l], ["gcol"])
        dma_sp(convw[:], convw_in[l], ["convw"])
        dma_sp(convp[:], convp_in[l], ["convp"])
        dma_sp(qkg[:], qkg_in[l], ["qkg"])
        P.op("pool", lambda h: h.dma_start(out=bias2[:], in_=bias2_in[l]), writes=["bias2"], dma=("b2", 0))
        if l % 2 == 1:
            dma_sp(wr[:], wr_in[l // 2], ["wr"])
            dma_sp(brb[:], br_in[l // 2], ["brb"])

    def mod_slab(l, sl):
        modcol = modcols[l % 2]
        mk = ("modcol", l % 2)
        wv = w_ada[l].rearrange("(k p) f -> p k f", p=128)
        colps = ps[7]
        t, key = load_slab(wv[:, :, sl * 512:(sl + 1) * 512], 8, 512)
        rb = sl % 2
        dma_sp(brow[0:1, rb, :], b_ada[l:l + 1, sl * 512:(sl + 1) * 512], [("brow", rb)])
        pr = ps[sl % 2]
        for k in range(8):
            mm(pr[0:1, :], cact[:, k:k + 1], t[:, k, :], k == 0, k == 7, [key, "cact"], [("ps", sl % 2)])
        dve(lambda h: h.tensor_tensor(out=rowb[0:1, rb, :], in0=pr[0:1, :], in1=brow[0:1, rb, :], op=ALU.add),
            [("ps", sl % 2), ("brow", rb)], [("rowb", rb)])
        v = sl // 2
        half = sl % 2
        if v in (2, 5):
            gi = 0 if v == 2 else 1
            pb = ps[2 + sl % 2]
            mm(pb[:, :], ones_f[0:1, :], rowb[0:1, rb, :], True, True, [("rowb", rb), "ones_f"], [("ps", 2 + sl % 2)])
            actop(lambda h: h.activation(out=gbc[:, gi, half * 512:(half + 1) * 512], in_=pb[:, :], func=AF.Identity),
                  [("ps", 2 + sl % 2)], [("gbc", gi)])
        else:
            vi = {0: 0, 1: 1, 3: 2, 4: 3}[v]
            for cc in range(4):
                mm(colps[:, cc:cc + 1], rowb[0:1, rb, cc * 128:(cc + 1) * 128], one_f, True, True,
                   [("rowb", rb), "ones_f"], [("ps", 7)])
            dve(lambda h: h.tensor_copy(out=modcol[:, vi, half * 4:(half + 1) * 4], in_=colps[:, 0:4]), [("ps", 7)], [mk])

    def mod_finish(l, parts=(0, 1)):
        modcol = modcols[l % 2]
        ABcol = ABcols[l % 2]
        for i, sci in ((0, 1), (1, 3)):
            if i not in parts:
                continue
            dve(lambda h, i=i, sci=sci: h.scalar_tensor_tensor(out=ABcol[:, i, :], in0=modcol[:, sci, :], scalar=1.0, in1=gcol[:, i, :],
                                                             op0=ALU.add, op1=ALU.mult), [("modcol", l % 2), "gcol"], [("ABcol", l % 2)])

    def norm_tile(t, which, router, dst=None):
        shi = 0 if which == 0 else 2
        modcol = modcols[LCUR[0] % 2]
        ABcol = ABcols[LCUR[0] % 2]
        mkeys = [("ABcol", LCUR[0] % 2), ("modcol", LCUR[0] % 2)]
        for ss_ in range(4):
            s = 4 * t + ss_
            if dst is None:
                dT, doff, dkey = hT, ss_ * 128, ("hT", ss_)
            else:
                dT, doff, dkey = dst, s * 128, ("h2", s)
            xb = ss_ % 2
            xn = A(xb * 1024, 1024)
            st = small[:, xb:xb + 1]
            st2 = small[:, 2 + xb:3 + xb]
            actop(lambda h, xn=xn, s=s, st=st: h.activation(out=xn, in_=x_tok[:, s, :], func=AF.Square, accum_out=st),
                  [("x", s)], [("xn", xb), ("st", xb)])
            actop(lambda h, st=st, st2=st2: h.activation(out=st2, in_=st, func=AF.Sqrt, scale=1.0 / D, bias=epsc[:, 0:1]),
                  [("st", xb)], [("st2", xb)])
            dve(lambda h, st2=st2: h.reciprocal(out=st2, in_=st2), [("st2", xb)], [("st2", xb)])
            actop(lambda h, xn=xn, s=s, st2=st2: h.activation(out=xn, in_=x_tok[:, s, :], func=AF.Identity, scale=st2),
                  [("x", s), ("st2", xb)], [("xn", xb)])
            h2f = A(2048, 1024).rearrange("p (c t) -> p c t", c=8)
            for hb in range(2):
                pb = ps[(2 * ss_ + hb) % 4]
                pk = ("ps", (2 * ss_ + hb) % 4)
                for cc in range(4):
                    c = hb * 4 + cc
                    mm(pb[:, cc * 128:(cc + 1) * 128], xn[:, c * 128:(c + 1) * 128], ident_f[:], True, True, [("xn", xb)], [pk])
                for cc in range(4):
                    c = hb * 4 + cc
                    if router:
                        actop(lambda h, pb=pb, cc=cc, c=c: h.activation(out=h2f[:, c, :], in_=pb[:, cc * 128:(cc + 1) * 128], func=AF.Identity,
                                                                       scale=ABcol[:, which, c:c + 1], bias=modcol[:, shi, c:c + 1]),
                              [pk] + mkeys, [("h2f", c)])
                    else:
                        actop(lambda h, pb=pb, cc=cc, c=c, dT=dT, doff=doff: h.activation(out=dT[:, c, doff:doff + 128], in_=pb[:, cc * 128:(cc + 1) * 128],
                                                                               func=AF.Identity, scale=ABcol[:, which, c:c + 1], bias=modcol[:, shi, c:c + 1]),
                              [pk] + mkeys, [dkey])
            if router:
                xnb = kvact[:, 0:16384].rearrange("p (s d) -> p s d", s=16)
                dve(lambda h, xn=xn, s=s: h.tensor_copy(out=xnb[:, s, :], in_=xn), [("xn", xb)], [("xnb", s)])
                route_subtile(s)

    def route_subtile(s):
        h2f = A(2048, 1024).rearrange("p (c t) -> p c t", c=8)
        pl = ps[4 + s % 2]
        pk = ("ps", 4 + s % 2)
        for c in range(8):
            mm(pl[:, 0:NE], h2f[:, c, :], wr[:, c, :], c == 0, c == 7, [("h2f", c), "wr"], [pk])
        lg = small[:, 8:16]
        m1 = small[:, 16:17]
        m2 = small[:, 17:18]
        nm1 = small[:, 18:19]
        den = small[:, 19:20]
        l2 = small[:, 28:36]
        ex = small[:, 36:44]
        sel = small[:, 44:52]
        dve(lambda h: h.tensor_tensor(out=lg, in0=pl[:, 0:NE], in1=brb[:], op=ALU.add), [pk, "brb"], ["lg"])
        dve(lambda h: h.tensor_reduce(out=m1, in_=lg, axis=AX.X, op=ALU.max), ["lg"], ["m1"])
        dve(lambda h: h.tensor_scalar(out=l2, in0=lg, scalar1=m1, scalar2=-1e30, op0=ALU.is_equal, op1=ALU.mult), ["lg", "m1"], ["l2"])
        dve(lambda h: h.tensor_tensor(out=l2, in0=l2, in1=lg, op=ALU.add), ["l2", "lg"], ["l2"])
        dve(lambda h: h.tensor_reduce(out=m2, in_=l2, axis=AX.X, op=ALU.max), ["l2"], ["m2"])
        dve(lambda h: h.tensor_scalar(out=nm1, in0=m1, scalar1=-1.0, scalar2=None, op0=ALU.mult), ["m1"], ["nm1"])
        actop(lambda h: h.activation(out=ex, in_=lg, func=AF.Exp, bias=nm1), ["lg", "nm1"], ["ex"])
        dve(lambda h: h.scalar_tensor_tensor(out=sel, in0=lg, scalar=m2, in1=ex, op0=ALU.is_ge, op1=ALU.mult), ["lg", "m2", "ex"], ["sel"])
        dve(lambda h: h.tensor_reduce(out=den, in_=sel, axis=AX.X, op=ALU.add), ["sel"], ["den"])
        dve(lambda h: h.reciprocal(out=den, in_=den), ["den"], ["den"])
        dve(lambda h: h.tensor_scalar(out=comb[:, s, :], in0=sel, scalar1=den, scalar2=None, op0=ALU.mult), ["sel", "den"], [("comb", s)])

    def proj_tile(l, t):
        wv = w_in[l].rearrange("(k p) f -> p k f", p=128)
        sa, ka = load_slab(wv[:, :, 0:512], 8, 512)
        sg_, kg_ = load_slab(wv[:, :, 512:1024], 8, 512)
        for c in range(4):
            pa, pka = ps[c % 2], ("ps", c % 2)
            pg, pkg = ps[2 + c % 2], ("ps", 2 + c % 2)
            for k in range(8):
                mm(pa[:, :], sa[:, k, c * 128:(c + 1) * 128], hT[:, k, :], k == 0, k == 7, [ka] + [("hT", i) for i in range(4)], [pka])
            for k in range(8):
                mm(pg[:, :], sg_[:, k, c * 128:(c + 1) * 128], hT[:, k, :], k == 0, k == 7, [kg_] + [("hT", i) for i in range(4)], [pkg])
            sgt = A(3072 + (c % 2) * 512, 512)
            actop(lambda h, pg=pg, sgt=sgt: h.activation(out=sgt, in_=pg[:, :], func=AF.Sigmoid), [pkg], [("sgt", c % 2)])
            dve(lambda h, pa=pa, sgt=sgt, c=c: h.tensor_tensor(out=zb[:, c, 30:542], in0=pa[:, :], in1=sgt, op=ALU.mult),
                [pka, ("sgt", c % 2)], [("zb", c)])
        for qi in range(2):
            sw, kw = load_slab(wv[:, :, 1024 + qi * 512:1536 + qi * 512], 8, 512)
            sqs = [A(3072 + c * 512, 512) for c in range(4)]
            for c in range(4):
                for k in range(8):
                    mm(ps[c][:, :], sw[:, k, c * 128:(c + 1) * 128], hT[:, k, :], k == 0, k == 7, [kw] + [("hT", i) for i in range(4)], [("ps", c)])
            for c in range(4):
                actop(lambda h, c=c: h.activation(out=sqs[c], in_=ps[c][:, :], func=AF.Square), [("ps", c)], [("sgt", c)])
            for c in range(4):
                mm(ps[4 + c][:, :], bdiag_f[:], sqs[c], True, True, [("sgt", c)], [("ps", 4 + c)])
            for c in range(4):
                actop(lambda h, c=c: h.activation(out=sqs[c], in_=ps[4 + c][:, :], func=AF.Sqrt, scale=1.0 / 64, bias=epsc[:, 0:1]),
                      [("ps", 4 + c)], [("sgt", c)])
            for c in range(4):
                dve(lambda h, c=c: h.reciprocal(out=sqs[c], in_=sqs[c]), [("sgt", c)], [("sgt", c)])
            for c in range(4):
                if qi == 0:
                    dst = qT[:, c, :]
                    dk = ("qT", c)
                else:
                    dst = kT[:, c, t * 512:(t + 1) * 512]
                    dk = ("kT", c, t)
                dve(lambda h, c=c, dst=dst, qi=qi: h.scalar_tensor_tensor(out=dst, in0=ps[c][:, :], scalar=small[:, 64 + qi:65 + qi], in1=sqs[c],
                                                                        op0=ALU.mult, op1=ALU.mult),
                    [("ps", c), ("sgt", c), "qkg8"], [dk])
        sv, kv = load_slab(wv[:, :, 2048:2560], 8, 512)
        for ss_ in range(4):
            pv, pkv = ps[ss_ % 2], ("ps", ss_ % 2)
            for k in range(8):
                mm(pv[:, :], hT[:, k, ss_ * 128:(ss_ + 1) * 128], sv[:, k, :], k == 0, k == 7, [kv, ("hT", ss_)], [pkv])
            actop(lambda h, pv=pv, ss_=ss_: h.activation(out=v_tok[:, 4 * t + ss_, :], in_=pv[:, :], func=AF.Identity), [pkv], [("v", 4 * t + ss_)])

    def conv_tile(l, t):
        cv = A(0, 2048).rearrange("p (c t) -> p c t", c=4)
        diag = AB(2048, 31 * 128).rearrange("p (a b) -> p a b", a=31)
        sqb = [A(4096, 512), A(4608, 512)]
        t1b = [A(5120, 512), A(5632, 512)]
        mean = A(6144, 512)
        rstd = A(6656, 512)
        msq = A(7168, 512)
        for c in range(4):
            dve(lambda h, c=c: h.tensor_tensor(out=diag, in0=ident_b[:].unsqueeze(1).to_broadcast([128, 31, 128]),
                                               in1=convw[:, c, :].unsqueeze(2).to_broadcast([128, 31, 128]), op=ALU.mult),
                ["convw"], ["diag"])
            pc, pkc = ps[c], ("ps", c)
            for tap in range(31):
                mm(pc[:, :], diag[:, tap, :], zb[:, c, tap:tap + 512], tap == 0, tap == 30, ["diag", ("zb", c)], [pkc])
            actop(lambda h, pc=pc, c=c: h.activation(out=cv[:, c, :], in_=pc[:, :], func=AF.Identity, bias=convp[:, 0, c:c + 1]),
                  [pkc, "convp"], [("cv", c)])
            sq = sqb[c % 2]
            actop(lambda h, sq=sq, c=c: h.activation(out=sq, in_=cv[:, c, :], func=AF.Square), [("cv", c)], [("sq", c % 2)])
            mm(ps[4][:, :], ones_f[:], cv[:, c, :], c == 0, c == 3, [("cv", c)], [("ps", 4)])
            mm(ps[5][:, :], ones_f[:], sq, c == 0, c == 3, [("sq", c % 2)], [("ps", 5)])
        dve(lambda h: h.tensor_copy(out=zb[:, :, 0:30], in_=zb[:, :, 512:542]), [("zb", c) for c in range(4)], [("zb", c) for c in range(4)])
        dve(lambda h: h.tensor_scalar(out=mean, in0=ps[4][:, :], scalar1=1.0 / 512, scalar2=None, op0=ALU.mult), [("ps", 4)], ["mean"])
        dve(lambda h: h.tensor_tensor(out=msq, in0=mean, in1=mean, op=ALU.mult), ["mean"], ["msq"])
        dve(lambda h: h.scalar_tensor_tensor(out=rstd, in0=ps[5][:, :], scalar=1.0 / 512, in1=msq, op0=ALU.mult, op1=ALU.subtract),
            [("ps", 5), "msq"], ["rstd"])
        actop(lambda h: h.activation(out=rstd, in_=rstd, func=AF.Sqrt, bias=epsc[:, 0:1]), ["rstd"], ["rstd"])
        dve(lambda h: h.reciprocal(out=rstd, in_=rstd), ["rstd"], ["rstd"])
        for c in range(4):
            t1 = t1b[c % 2]
            dve(lambda h, t1=t1, c=c: h.tensor_tensor(out=t1, in0=cv[:, c, :], in1=mean, op=ALU.subtract), [("cv", c), "mean"], [("t1", c % 2)])
            dve(lambda h, t1=t1: h.tensor_tensor(out=t1, in0=t1, in1=rstd, op=ALU.mult), [("t1", c % 2), "rstd"], [("t1", c % 2)])
            actop(lambda h, t1=t1, c=c: h.activation(out=ycat[:, c, :], in_=t1, func=AF.Silu, scale=convp[:, 1, c:c + 1], bias=convp[:, 2, c:c + 1]),
                  [("t1", c % 2), "convp"], [("ycat", c)])

    def attn_tile(l, t):
        pb_ = [AB(5120, 640), AB(5440, 640)]
        pTb = [AB(5760, 640), AB(6080, 640)]
        yatt_sb = AB(6400, 512)
        rs_alls = [small[:, 66:74], small[:, 82:90]]
        rinv = small[:, 20:28]
        pTt = psd[2]
        kPT = [("ps", 4), ("ps", 5)]
        blocks = []
        for pi in range(4):
            i = 4 * t + pi
            c0 = max(0, 2 * i - 8)
            k0 = c0 * 64
            nk = (2 * i + 2 - c0) * 64
            for hh in range(NH):
                blocks.append(dict(pi=pi, hh=hh, k0=k0, nk=nk, boff=640 - nk, q0=pi * 128, nkc=nk // 128))

        def stage_A(b):
            B_ = blocks[b]
            pi, hh, k0, nk, boff, q0 = B_["pi"], B_["hh"], B_["k0"], B_["nk"], B_["boff"], B_["q0"]
            c, hp = hh // 2, (hh % 2) * 64
            bb = b % 2
            pS = psd[bb]
            kS = [("ps", 2 * bb), ("ps", 2 * bb + 1)]
            n0 = min(nk, 512)
            kreads = [("kT", c, tt) for tt in range(k0 // 512, (k0 + nk - 1) // 512 + 1)]
            mm(pS[:, 0:n0], qT[hp:hp + 64, c, q0:q0 + 128], kT[hp:hp + 64, c, k0:k0 + n0], True, False, [("qT", c)] + kreads, kS)
            mm(pS[:, 0:n0], ident_b[:], bias2[:, hh, boff:boff + n0], False, True, ["bias2"], kS)
            if nk > 512:
                mm(pS[:, 512:nk], qT[hp:hp + 64, c, q0:q0 + 128], kT[hp:hp + 64, c, k0 + 512:k0 + nk], True, False, [("qT", c)] + kreads, kS)
                mm(pS[:, 512:nk], ident_b[:], bias2[:, hh, boff + 512:boff + nk], False, True, ["bias2"], kS)
            mx = small[:, 74 + bb:75 + bb]
            dve(lambda h: h.tensor_reduce(out=mx, in_=pS[:, 0:nk], axis=AX.X, op=ALU.max, negate=True), kS, [("mx", bb)])
            pp = pb_[bb]
            rs = rs_alls[pi % 2]
            actop(lambda h: h.activation(out=pp[:, 0:nk], in_=pS[:, 0:nk], func=AF.Exp, bias=mx, accum_out=rs[:, hh:hh + 1]),
                  kS + [("mx", bb)], [("p", bb), ("rs_all", pi % 2, hh)])

        def stage_B(b):
            B_ = blocks[b]
            nk, nkc = B_["nk"], B_["nkc"]
            bb = b % 2
            pp = pb_[bb]
            for kc in range(nkc):
                mm(pTt[:, kc * 128:(kc + 1) * 128], pp[:, kc * 128:(kc + 1) * 128], ident_b[:], True, True, [("p", bb)], kPT)
            pT = pTb[bb]
            if b % 2 == 0:
                actop(lambda h: h.activation(out=pT[:, 0:nk], in_=pTt[:, 0:nk], func=AF.Identity), kPT, [("pT", bb)])
            else:
                dve(lambda h: h.tensor_copy(out=pT[:, 0:nk], in_=pTt[:, 0:nk]), kPT, [("pT", bb)])

        def stage_C(b):
            B_ = blocks[b]
            pi, hh, k0, nkc = B_["pi"], B_["hh"], B_["k0"], B_["nkc"]
            bb = b % 2
            pT = pTb[bb]
            yb = 6 + pi % 2
            for kc in range(nkc):
                mm(ps[yb][:, hh * 64:(hh + 1) * 64], pT[:, kc * 128:(kc + 1) * 128], v_tok[:, k0 // 128 + kc, hh * 64:(hh + 1) * 64],
                   kc == 0, kc == nkc - 1, [("pT", bb), ("v", k0 // 128 + kc)], [("ps", yb)])

        def finalize(pi):
            q0 = pi * 128
            yb = 6 + pi % 2
            rs = rs_alls[pi % 2]
            dve(lambda h: h.reciprocal(out=rinv, in_=rs), [("rs_all", pi % 2, hh) for hh in range(NH)], ["rinv"])
            dve(lambda h: h.tensor_tensor(out=yatt_sb.rearrange("p (a b) -> p a b", a=NH), in0=ps[yb][:, :].rearrange("p (a b) -> p a b", a=NH),
                                          in1=rinv.unsqueeze(2).to_broadcast([128, NH, 64]), op=ALU.mult), [("ps", yb), "rinv"], ["yatt_sb"])
            for cc in range(4):
                mm(pTt[:, cc * 128:(cc + 1) * 128], yatt_sb[:, cc * 128:(cc + 1) * 128], ident_b[:], True, True, ["yatt_sb"], kPT)
            actop(lambda h: h.activation(out=ycat[:, 4:8, q0:q0 + 128], in_=pTt[:, 0:512].rearrange("p (a b) -> p a b", a=4), func=AF.Identity),
                  kPT, [("ycat", 4 + cc) for cc in range(4)])

        nb = len(blocks)
        stage_A(0)
        stage_A(1)
        for b in range(nb):
            stage_B(b)
            if b + 2 < nb:
                stage_A(b + 2)
            stage_C(b)
            if blocks[b]["hh"] == NH - 1:
                finalize(blocks[b]["pi"])

    def outproj_tile(l, t):
        wv = w_out[l].rearrange("(k p) f -> p k f", p=128)
        tmpb = [A(6656, 512), A(7168, 512)]
        n = 0
        for half in range(2):
            sw, kw = load_slab(wv[:, :, half * 512:(half + 1) * 512], 8, 512)
            for ss_ in range(4):
                s = 4 * t + ss_
                po, pko = ps[n % 2], ("ps", n % 2)
                tb = tmpb[n % 2]
                for k in range(8):
                    mm(po[:, :], ycat[:, k, ss_ * 128:(ss_ + 1) * 128], sw[:, k, :], k == 0, k == 7, [kw, ("ycat", k)], [pko])
                dve(lambda h, po=po, tb=tb, half=half: h.tensor_tensor(out=tb, in0=po[:, :], in1=gbc[:, 0, half * 512:(half + 1) * 512], op=ALU.mult),
                    [pko, ("gbc", 0)], [("tmp", n % 2)])
                dve(lambda h, tb=tb, s=s, half=half: h.tensor_tensor(out=x_tok[:, s, half * 512:(half + 1) * 512], in0=x_tok[:, s, half * 512:(half + 1) * 512], in1=tb, op=ALU.add),
                    [("tmp", n % 2), ("x", s)], [("x", s)])
                n += 1

    def ffn_item(t, wg, wu, wd, nj, e):
        wgv = wg.rearrange("(k p) f -> p k f", p=128)
        wuv = wu.rearrange("(k p) f -> p k f", p=128)
        wdv = wd.rearrange("(j p) d -> p j d", p=128)
        sgb = [A(0, 512), A(512, 512)]
        tmpb = [A(1024, 512), A(1536, 512)]
        hreads = [("hT", i) for i in range(4)]
        nslab = (nj + 3) // 4
        for sl in range(nslab):
            j0 = sl * 4
            njs = min(4, nj - j0)
            sgw, kgw = load_slab(wgv[:, :, j0 * 128:(j0 + njs) * 128], 8, njs * 128)
            suw, kuw = load_slab(wuv[:, :, j0 * 128:(j0 + njs) * 128], 8, njs * 128)
            for jj in range(njs):
                j = j0 + jj
                pg, pkg = ps[j % 2], ("ps", j % 2)
                pu, pku = ps[2 + j % 2], ("ps", 2 + j % 2)
                for k in range(8):
                    mm(pg[:, :], sgw[:, k, jj * 128:(jj + 1) * 128], hT[:, k, :], k == 0, k == 7, [kgw] + hreads, [pkg])
                for k in range(8):
                    mm(pu[:, :], suw[:, k, jj * 128:(jj + 1) * 128], hT[:, k, :], k == 0, k == 7, [kuw] + hreads, [pku])
                sg = sgb[j % 2]
                actop(lambda h, sg=sg, pg=pg: h.activation(out=sg, in_=pg[:, :], func=AF.Silu), [pkg], [("sg", j % 2)])
                dve(lambda h, sg=sg, pu=pu, j=j: h.tensor_tensor(out=act[:, j, :], in0=pu[:, :], in1=sg, op=ALU.mult), [pku, ("sg", j % 2)], [("act", j)])
        nds = (nj + 7) // 8
        n = 0
        for half in range(2):
            for sl in range(nds):
                j0 = sl * 8
                njs = min(8, nj - j0)
                sdw, kdw = load_slab(wdv[:, j0:j0 + njs, half * 512:(half + 1) * 512], njs, 512)
                for jj in range(njs):
                    j = j0 + jj
                    for ss_ in range(4):
                        mm(ps[4 + ss_][:, :], act[:, j, ss_ * 128:(ss_ + 1) * 128], sdw[:, jj, :], j == 0, j == nj - 1, [kdw, ("act", j)], [("ps", 4 + ss_)])
            for ss_ in range(4):
                s = 4 * t + ss_
                tb = tmpb[n % 2]
                po = ps[4 + ss_]
                if e is None:
                    dve(lambda h, po=po, tb=tb, half=half: h.tensor_tensor(out=tb, in0=po[:, :], in1=gbc[:, 1, half * 512:(half + 1) * 512], op=ALU.mult),
                        [("ps", 4 + ss_), ("gbc", 1)], [("tmp", n % 2)])
                else:
                    dve(lambda h, po=po, tb=tb, half=half, s=s, e=e: h.scalar_tensor_tensor(out=tb, in0=po[:, :], scalar=comb[:, s, e:e + 1], in1=gbc[:, 1, half * 512:(half + 1) * 512],
                                                                                    op0=ALU.mult, op1=ALU.mult),
                        [("ps", 4 + ss_), ("gbc", 1), ("comb", s)], [("tmp", n % 2)])
                dve(lambda h, tb=tb, s=s, half=half: h.tensor_tensor(out=x_tok[:, s, half * 512:(half + 1) * 512], in0=x_tok[:, s, half * 512:(half + 1) * 512], in1=tb, op=ALU.add),
                    [("tmp", n % 2), ("x", s)], [("x", s)])
                n += 1

    def flat_of(tn):
        return tn[:].rearrange("p a b -> p (a b)")
    ffn_ring = [(flat_of(slabs[i]), ("slab", i)) for i in range(NSLAB)]
    ffn_ring.append((arena[:, 3072:5120].bitcast(BF16), ("slab", NSLAB)))
    ffn_ring.append((arena[:, 5120:7168].bitcast(BF16), ("slab", NSLAB + 1)))
    ffn_i = [0]

    def load_ffn(src_ap, down, n0, n1):
        flat, key = ffn_ring[ffn_i[0] % len(ffn_ring)]
        ffn_i[0] += 1
        if down:
            view = flat.rearrange("p (j d) -> p j d", j=4)
        else:
            view = flat.rearrange("p (k f) -> p k f", k=8)
        rd = ["ffn_go"] if key[1] >= NSLAB else []
        P.op("pool", lambda h: h.dma_start(out=view[:, 0:n0, 0:n1], in_=src_ap), reads=rd, writes=[key], dma=key)
        return view, key

    def ffn_experts(wts, nj, hook=None):
        h2all = kvact[:, 0:16384].rearrange("p (c t) -> p c t", c=8)
        actv = [flat_of(hT).rearrange("p (j t) -> p j t", j=2), flat_of(ycat).rearrange("p (j t) -> p j t", j=2)]
        sgb = [A(0, 512), A(512, 512)]
        tmpb = [A(1024, 512), A(1536, 512)]
        n = 0
        m = 0
        for (wg, wu, wd, e) in wts:
            wgv = wg.rearrange("(k p) f -> p k f", p=128)
            wuv = wu.rearrange("(k p) f -> p k f", p=128)
            wdv = wd.rearrange("(j p) d -> p j d", p=128)
            for sl in range((nj + 3) // 4):
                j0 = sl * 4
                njs = min(4, nj - j0)
                if hook is not None:
                    hook()
                sgw, kgw = load_ffn(wgv[:, :, j0 * 128:(j0 + njs) * 128], False, 8, njs * 128)
                suw, kuw = load_ffn(wuv[:, :, j0 * 128:(j0 + njs) * 128], False, 8, njs * 128)
                sdw, kdw = load_ffn(wdv[:, j0:j0 + njs, :], True, njs, 1024)
                dve(lambda h, sdw=sdw, njs=njs: h.tensor_tensor(out=sdw[:, 0:njs, :], in0=sdw[:, 0:njs, :],
                                                                in1=gbc[:, 1, :].unsqueeze(1).to_broadcast([128, njs, 1024]), op=ALU.mult),
                    [kdw, ("gbc", 1)], [kdw])
                for t in range(NT):
                    hreads = [("h2", 4 * t + i) for i in range(4)]
                    for jj in range(njs):
                        pg, pkg = ps[n % 2], ("ps", n % 2)
                        pu, pku = ps[2 + n % 2], ("ps", 2 + n % 2)
                        sg = sgb[n % 2]
                        for k in range(8):
                            mm(pg[:, :], sgw[:, k, jj * 128:(jj + 1) * 128], h2all[:, k, t * 512:(t + 1) * 512], k == 0, k == 7, [kgw] + hreads, [pkg])
                        for k in range(8):
                            mm(pu[:, :], suw[:, k, jj * 128:(jj + 1) * 128], h2all[:, k, t * 512:(t + 1) * 512], k == 0, k == 7, [kuw] + hreads, [pku])
                        actop(lambda h, sg=sg, pg=pg: h.activation(out=sg, in_=pg[:, :], func=AF.Silu), [pkg], [("sg", n % 2)])
                        dst = actv[jj // 2][:, jj % 2, t * 512:(t + 1) * 512]
                        dve(lambda h, sg=sg, pu=pu, dst=dst: h.tensor_tensor(out=dst, in0=pu[:, :], in1=sg, op=ALU.mult), [pku, ("sg", n % 2)], [("act", jj, t)])
                        n += 1
                for s in range(NSUB):
                    for half in range(2):
                        po, pko = ps[4 + m % 4], ("ps", 4 + m % 4)
                        tb = tmpb[m % 2]
                        for jj in range(njs):
                            mm(po[:, :], actv[jj // 2][:, jj % 2, s * 128:(s + 1) * 128], sdw[:, jj, half * 512:(half + 1) * 512], jj == 0, jj == njs - 1,
                               [kdw, ("act", jj, s // 4)], [pko])
                        sc = 1.0 if e is None else comb[:, s, e:e + 1]
                        dve(lambda h, po=po, half=half, s=s, sc=sc: h.scalar_tensor_tensor(out=x_tok[:, s, half * 512:(half + 1) * 512], in0=po[:, :], scalar=sc,
                                                                                       in1=x_tok[:, s, half * 512:(half + 1) * 512], op0=ALU.mult, op1=ALU.add),
                            [pko, ("x", s), ("comb", s)], [("x", s)])
                        m += 1

    b2f = flat_of(bias2)
    gu_ring = [(flat_of(slabs[i]), ("slab", i)) for i in range(NSLAB)] + [(b2f[:, 0:4096], ("hs", 0))]
    dn_ring = [(flat_of(qT), ("hs", 1)), (flat_of(zb)[:, 0:2048], ("hs", 2)), (gbc[:, 0, :].bitcast(BF16), ("hs", 3))]
    gu_i = [0]
    dn_i = [0]
    moe_seen = set()

    def load_moe(src_ap, down):
        if down:
            flat, key = dn_ring[dn_i[0] % len(dn_ring)]
            dn_i[0] += 1
            view = flat.rearrange("p (j d) -> p j d", j=2)
        else:
            flat, key = gu_ring[gu_i[0] % len(gu_ring)]
            gu_i[0] += 1
            view = flat.rearrange("p (k f) -> p k f", k=8)
        rd = []
        if key not in moe_seen:
            moe_seen.add(key)
            rd = ["ffn_go"]
        P.op("pool", lambda h: h.dma_start(out=view, in_=src_ap), reads=rd, writes=[key], dma=key)
        return view, key

    R_SEL, R_POSM, R_FF, R_FLI = 6144, 6272, 6400, 6448
    MOE_CHUNKS = [(0, 512)] + [(512 + 256 * i, 256) for i in range(6)]
    NFLAG = len(MOE_CHUNKS) - 1

    def moe_route_post():
        combf = comb[:].rearrange("p s e -> p (s e)")
        self_ = A(R_SEL, 128)
        selv = self_.rearrange("p (s e) -> p s e", s=NSUB)
        posm = A(R_POSM, 128)
        ff = A(R_FF, NFLAG * 8)
        fli = arena[:, R_FLI:R_FLI + NFLAG * 8].bitcast(I32)
        dve(lambda h: h.tensor_scalar(out=self_, in0=combf, scalar1=0.0, scalar2=None, op0=ALU.is_gt), [("comb", s) for s in range(NSUB)], ["sel"])
        for s in range(NSUB):
            mm(ps[4][:, s * 8:(s + 1) * 8], ustrict[:], selv[:, s, :], True, s == 0, ["sel"], [("ps", 4)])
            for s2 in range(s):
                mm(ps[4][:, s * 8:(s + 1) * 8], ones_f[:], selv[:, s2, :], False, s2 == s - 1, ["sel"], [("ps", 4)])
        for s in range(NSUB):
            mm(ps[5][:, 0:NE], ones_f[:], selv[:, s, :], s == 0, s == NSUB - 1, ["sel"], [("ps", 5)])
        dve(lambda h: h.scalar_tensor_tensor(out=posm, in0=ps[4][:, 0:128], scalar=1.0, in1=self_, op0=ALU.add, op1=ALU.mult), [("ps", 4), "sel"], ["posm"])
        dve(lambda h: h.tensor_scalar(out=posm, in0=posm, scalar1=-1.0, scalar2=1.0, op0=ALU.mult, op1=ALU.add), ["posm"], ["posm"])
        for q in range(1, NFLAG + 1):
            dve(lambda h, q=q: h.tensor_scalar(out=ff[:, (q - 1) * 8:q * 8], in0=ps[5][:, 0:NE], scalar1=float(MOE_CHUNKS[q][0]) - 0.5, scalar2=None, op0=ALU.is_gt),
                [("ps", 5)], ["ff"])
        dve(lambda h: h.tensor_copy(out=fli, in_=ff), ["ff"], ["fli"])

    def moe_chunk(l, e, slot0, NS):
        NU = NS // 128
        wguv = moe_wgu[l // 2, e].rearrange("(k p) f -> p k f", p=128)
        wdv = moe_wd[l // 2, e].rearrange("(j p) d -> p j d", p=128)
        xnb = kvact[:, 0:16384].rearrange("p (s d) -> p s d", s=16)
        posm = A(R_POSM, 128).rearrange("p (s e) -> p s e", s=NSUB)
        hg = hT
        modcol = modcols[l % 2]
        ABcol = ABcols[l % 2]
        ybf = flat_of(hT).rearrange("p (u d) -> p u d", u=4)
        hgkeys = [("hg", c) for c in range(8)]
        sgb = [A(0, 512), A(512, 512)]
        Gb = [AB(1024, 512), AB(1280, 512)]
        GTb = [AB(1536, 512), AB(1792, 512)]
        yacc = A(2048, 4096).rearrange("p (u d) -> p u d", u=4)
        acts = flat_of(ycat)[:, 0:2048].rearrange("p (b j t) -> p b j t", b=2, j=2)
        gi = [0]

        def build_G(s):
            b = gi[0] % 2
            gi[0] += 1
            G = Gb[b]
            dve(lambda h: h.tensor_scalar(out=G[:, 0:NS], in0=iota_f[:, 0:NS], scalar1=posm[:, s, e:e + 1], scalar2=float(-slot0), op0=ALU.add, op1=ALU.is_equal),
                ["posm"], [("G", b)])
            return G, ("G", b)

        for pas in range(2):
            for s in range(NSUB):
                G, gk = build_G(s)
                for cc in range(4):
                    c = pas * 4 + cc
                    mm(ps[4 + cc][:, 0:NS], xnb[:, s, c * 128:(c + 1) * 128], G[:, 0:NS], s == 0, s == NSUB - 1, [gk, ("xnb", s)], [("ps", 4 + cc)])
            for cc in range(4):
                c = pas * 4 + cc
                actop(lambda h, cc=cc, c=c: h.activation(out=hg[:, c, 0:NS], in_=ps[4 + cc][:, 0:NS], func=AF.Identity,
                                                       scale=ABcol[:, 1, c:c + 1], bias=modcol[:, 2, c:c + 1]),
                      [("ps", 4 + cc), ("ABcol", l % 2), ("modcol", l % 2)], [("hg", c)])
        n = 0
        mc = [0]

        def down_step(st, ab, sdw, kdw):
            for u in range(NU):
                for half in range(2):
                    m = mc[0]
                    po, pko = ps[4 + m % 4], ("ps", 4 + m % 4)
                    for jj in range(2):
                        mm(po[:, :], acts[:, ab, jj, u * 128:(u + 1) * 128], sdw[:, jj, half * 512:(half + 1) * 512], jj == 0, jj == 1,
                           [kdw, ("act", ab, jj)], [pko])
                    ya = yacc[:, u, half * 512:(half + 1) * 512]
                    if st == 0:
                        dve(lambda h, po=po, ya=ya: h.tensor_copy(out=ya, in_=po[:, :]), [pko], [("yacc", u, half)])
                    else:
                        dve(lambda h, po=po, ya=ya: h.tensor_tensor(out=ya, in0=po[:, :], in1=ya, op=ALU.add), [pko, ("yacc", u, half)], [("yacc", u, half)])
                    mc[0] += 1

        pend = None
        for st in range(FF_EXP // 256):
            j0 = 2 * st
            sgu, kgu = load_moe(wguv[:, :, st * 512:(st + 1) * 512], False)
            sgw, kgw = sgu[:, :, 0:256], kgu
            suw, kuw = sgu[:, :, 256:512], kgu
            sdw, kdw = load_moe(wdv[:, j0:j0 + 2, :], True)
            ab = st % 2
            for jj in range(2):
                pg, pkg = ps[n % 2], ("ps", n % 2)
                pu, pku = ps[2 + n % 2], ("ps", 2 + n % 2)
                sg = sgb[n % 2]
                for k in range(8):
                    mm(pg[:, 0:NS], sgw[:, k, jj * 128:(jj + 1) * 128], hg[:, k, 0:NS], k == 0, k == 7, [kgw] + hgkeys, [pkg])
                for k in range(8):
                    mm(pu[:, 0:NS], suw[:, k, jj * 128:(jj + 1) * 128], hg[:, k, 0:NS], k == 0, k == 7, [kuw] + hgkeys, [pku])
                actop(lambda h, sg=sg, pg=pg: h.activation(out=sg[:, 0:NS], in_=pg[:, 0:NS], func=AF.Silu), [pkg], [("sg", n % 2)])
                dve(lambda h, sg=sg, pu=pu, ab=ab, jj=jj: h.tensor_tensor(out=acts[:, ab, jj, 0:NS], in0=pu[:, 0:NS], in1=sg[:, 0:NS], op=ALU.mult),
                    [pku, ("sg", n % 2)], [("act", ab, jj)])
                n += 1
            if pend is not None:
                down_step(*pend)
            pend = (st, ab, sdw, kdw)
        down_step(*pend)
        m = mc[0]
        for u in range(NU):
            dve(lambda h, u=u: h.tensor_tensor(out=ybf[:, u, :], in0=yacc[:, u, :], in1=gbc[:, 1, :], op=ALU.mult),
                [("yacc", u, 0), ("yacc", u, 1), ("gbc", 1)], hgkeys)
        Gs = {}

        def ensure_G(s):
            if s < NSUB and s not in Gs:
                Gs[s] = build_G(s)

        def emit_T(s):
            G, gk = Gs[s]
            pt, pkt = ps[s % 2], ("ps", s % 2)
            for u in range(NU):
                mm(pt[:, u * 128:(u + 1) * 128], G[:, u * 128:(u + 1) * 128], ident_b[:], True, True, [gk], [pkt])
            GT = GTb[s % 2]
            actop(lambda h: h.activation(out=GT[:, 0:NS], in_=pt[:, 0:NS], func=AF.Identity), [pkt], [("GT", s % 2)])

        ensure_G(0)
        ensure_G(1)
        emit_T(0)
        for s in range(NSUB):
            if s + 1 < NSUB:
                emit_T(s + 1)
            ensure_G(s + 2)
            GT = GTb[s % 2]
            for half in range(2):
                po, pko = ps[4 + m % 4], ("ps", 4 + m % 4)
                for u in range(NU):
                    mm(po[:, :], GT[:, u * 128:(u + 1) * 128], ybf[:, u, half * 512:(half + 1) * 512], u == 0, u == NU - 1, [("GT", s % 2)] + hgkeys, [pko])
                dve(lambda h, po=po, half=half, s=s: h.scalar_tensor_tensor(out=x_tok[:, s, half * 512:(half + 1) * 512], in0=po[:, :], scalar=comb[:, s, e:e + 1],
                                                                          in1=x_tok[:, s, half * 512:(half + 1) * 512], op0=ALU.mult, op1=ALU.add),
                    [pko, ("x", s), ("comb", s)], [("x", s)])
                m += 1

    def moe_layer(l):
        dve(lambda h: h.memset(small[:, 80:81], 0.0), [], ["ffn_go"])
        moe_seen.clear()
        for t in range(NT):
            norm_tile(t, 1, True)
        P.barrier()
        moe_route_post()
        fli = arena[:, R_FLI:R_FLI + NFLAG * 8].bitcast(I32)
        for e in range(NE):
            moe_chunk(l, e, MOE_CHUNKS[0][0], MOE_CHUNKS[0][1])
        for e in range(NE):
            for q, (slot0, nsl) in enumerate(MOE_CHUNKS):
                if q > 0:
                    P.begin_region(fli[0:1, (q - 1) * 8 + e:(q - 1) * 8 + e + 1])
                    moe_chunk(l, e, slot0, nsl)
            for q in range(1, len(MOE_CHUNKS)):
                P.end_region()
        P.ops.append(("barrier_all",))

    def ffn_layer(l):
        moe = (l % 2 == 1)
        if moe and SPARSE_MOE:
            moe_layer(l)
            return
        h2all = kvact[:, 0:16384].rearrange("p (c t) -> p c t", c=8)
        dve(lambda h: h.memset(small[:, 80:81], 0.0), [], ["ffn_go"])
        for t in range(NT):
            norm_tile(t, 1, moe, dst=h2all)
        P.barrier()
        if moe:
            wts = [(moe_wg[l // 2, e], moe_wu[l // 2, e], moe_wd[l // 2, e], e) for e in range(NE)]
            ffn_experts(wts, FF_EXP // 128)
        else:
            hook = None
            if l + 1 in layers:
                todo = list(range(10))

                def hook():
                    for _ in range(2):
                        if todo:
                            mod_slab(l + 1, todo.pop(0))
            ffn_experts([(ffn_wg[l // 2], ffn_wu[l // 2], ffn_wg[l // 2] if False else ffn_wd[l // 2], None)], FF_DENSE // 128, hook)
            if l + 1 in layers:
                while todo:
                    mod_slab(l + 1, todo.pop(0))
                PRE_MOD.add(l + 1)
        P.barrier()

    PRE_MOD = set()
    for l in layers:
        LCUR[0] = l
        layer_consts(l)
        dve(lambda h: h.memset(zb[:, :, 0:30], 0.0), [], [("zb", c) for c in range(4)])
        if l in PRE_MOD:
            for sl in range(10, 12):
                mod_slab(l, sl)
            mod_finish(l)
            later = []
        else:
            for sl in range(4):
                mod_slab(l, sl)
            mod_finish(l, parts=(0,))
            later = list(range(4, 12))
        dve(lambda h: h.tensor_scalar(out=small[:, 64:65], in0=qkg[:, 0:1], scalar1=0.125, scalar2=None, op0=ALU.mult), ["qkg"], ["qkg8"])
        dve(lambda h: h.tensor_scalar(out=small[:, 65:66], in0=qkg[:, 1:2], scalar1=1.0, scalar2=None, op0=ALU.mult), ["qkg"], ["qkg8"])
        P.barrier()
        for t in range(NT):
            norm_tile(t, 0, False)
            if debug and "hT" in dbg_out:
                P.op("pool", lambda h, t=t: h.dma_start(out=dbg_out["hT"][t], in_=hT[:]), reads=[("hT", i) for i in range(4)], writes=[("dbg", 0)], dma=("dbg", 0))
            proj_tile(l, t)
            for _ in range(4):
                if later:
                    mod_slab(l, later.pop(0))
            P.barrier()
            conv_tile(l, t)
            P.barrier()
            if later and later[0] < 6:
                raise AssertionError("g1 slabs must precede the out-projection")
            attn_tile(l, t)
            for _ in range(4):
                if later:
                    mod_slab(l, later.pop(0))
            if later == [] and t == 0 and l not in PRE_MOD:
                mod_finish(l, parts=(1,))
            if debug and "ycat" in dbg_out:
                P.op("pool", lambda h, t=t: h.dma_start(out=dbg_out["ycat"][t], in_=ycat[:]), reads=[("ycat", i) for i in range(8)], writes=[("dbg", 1)], dma=("dbg", 1))
            outproj_tile(l, t)
        P.barrier()
        if debug and "xmid" in dbg_out:
            P.op("sp", lambda h: h.dma_start(out=dbg_out["xmid"].rearrange("(s p) d -> p s d", p=128), in_=x_tok[:]), reads=[("x", i) for i in range(16)], writes=[("dbg", 2)], dma=("dbg", 2))
        ffn_layer(l)

    yv = y_out.rearrange("(s p) d -> p s d", p=128)
    for s4 in range(4):
        P.op("sp", lambda h, s4=s4: h.dma_start(out=yv[:, 4 * s4:4 * s4 + 4, :], in_=x_tok[:, 4 * s4:4 * s4 + 4, :]),
             reads=[("x", 4 * s4 + i) for i in range(4)], writes=[("y", s4)], dma=("xio", s4))
    P.op("sp", None, reads=[("y", s4) for s4 in range(4)] + [("dbg", i) for i in range(3)], writes=[])

    P.emit(nc, stack)
    stack.close()
    return nc


def _bias2_index():
    q = np.arange(128)[:, None]
    j = np.arange(640)[None, :]
    rel = j - 512 - q
    idx = np.clip(rel, -128, 128) + 128
    valid = np.where(q < 64, j < 576, j >= 64)
    return idx, valid


def _interleave_gate_up(g, u):
    n, e, d, ff = g.shape
    st = ff // 256
    out = np.empty((n, e, d, st, 2, 256), np.float32)
    out[:, :, :, :, 0, :] = g.reshape(n, e, d, st, 256)
    out[:, :, :, :, 1, :] = u.reshape(n, e, d, st, 256)
    return out.reshape(n, e, d, 2 * ff)


def _host_layout(inp):
    f = np.float32
    idx, valid = _bias2_index()
    rel_bias = np.asarray(inp["rel_bias"], f)
    gathered = rel_bias[:, :, idx]
    bias2 = np.where(valid[None, None], gathered, f(MASK_NEG)).astype(f)
    bias2 = np.ascontiguousarray(bias2.transpose(0, 2, 1, 3))

    def col8(v):
        return np.ascontiguousarray(np.asarray(v, f).reshape(DEPTH, 8, 128).transpose(0, 2, 1))

    def col4(v):
        return np.asarray(v, f).reshape(DEPTH, 4, 128).transpose(0, 2, 1)

    convw = np.ascontiguousarray(np.asarray(inp["conv_w"], f)[:, :, 0, :].reshape(DEPTH, 31, 4, 128).transpose(0, 3, 2, 1))
    convp = np.ascontiguousarray(np.stack([col4(inp["conv_b"]), col4(inp["conv_ln_g"]), col4(inp["conv_ln_b"])], axis=2))
    qg = np.asarray(inp["q_norm_g"], f)
    kg = np.asarray(inp["k_norm_g"], f)
    qkg = np.ascontiguousarray(np.stack([np.tile(qg, (1, 2)), np.tile(kg, (1, 2))], axis=2))
    wr = np.ascontiguousarray(np.asarray(inp["moe_w_router"], f).reshape(1, 8, 128, NE).transpose(0, 2, 1, 3))
    br = np.ascontiguousarray(np.broadcast_to(np.asarray(inp["moe_b_router"], f)[:, None, :], (1, 128, NE)))
    shared = {
        "w_ada": np.ascontiguousarray(inp["w_ada"], f), "b_ada": np.ascontiguousarray(inp["b_ada"], f),
        "gmix": col8(inp["norm_mix_g"]), "gffn": col8(inp["norm_ffn_g"]),
        "w_in": np.ascontiguousarray(inp["w_in"], f), "w_out": np.ascontiguousarray(inp["w_out"], f),
        "convw": convw, "convp": convp, "qkg": qkg, "bias2": bias2,
        "ffn_wg": np.ascontiguousarray(inp["ffn_w_gate"], f), "ffn_wu": np.ascontiguousarray(inp["ffn_w_up"], f),
        "ffn_wd": np.ascontiguousarray(inp["ffn_w_down"], f),
        "wr": wr, "br": br,
        "moe_wgu": _interleave_gate_up(np.asarray(inp["moe_w_gate"], f), np.asarray(inp["moe_w_up"], f)),
        "moe_wd": np.ascontiguousarray(inp["moe_w_down"], f),
        "ident": np.eye(128, dtype=f),
        "ustrict": np.triu(np.ones((128, 128), f), 1),
        "iota": np.ascontiguousarray(np.broadcast_to(np.arange(512, dtype=f)[None, :], (128, 512))),
    }
    c = np.asarray(inp["c"], f)
    cT = [np.ascontiguousarray(c[b].reshape(8, 128).T) for b in range(8)]
    return shared, cT


LAUNCH_GROUPS = [[0, 1]]
_PROG_CACHE = {}


def kernel(**inputs):
    shared, cT = _host_layout(inputs)
    x = np.ascontiguousarray(np.asarray(inputs["x"], np.float32))
    cur = [x[b] for b in range(8)]
    for grp in LAUNCH_GROUPS:
        key = tuple(grp)
        if key not in _PROG_CACHE:
            _PROG_CACHE[key] = build_program(grp)
        nc = _PROG_CACHE[key]
        in_maps = []
        drop = set()
        if 1 not in grp:
            drop |= {"moe_wgu", "moe_wd"}
        if 0 not in grp:
            drop |= {"ffn_wg", "ffn_wu", "ffn_wd"}
        for b in range(8):
            m = {k: v for k, v in shared.items() if k not in drop}
            m["x"] = cur[b]
            m["cT"] = cT[b]
            in_maps.append(m)
        res = run_bass_kernel_spmd(nc, in_maps, core_ids=list(range(8)))
        cur = [np.asarray(res.results[b]["y"], np.float32) for b in range(8)]
    return np.stack(cur, axis=0)
```

```python
import numpy as np
from contextlib import ExitStack
import concourse.bass as bass
import concourse.mybir as mybir
from concourse.bass_utils import run_bass_kernel_spmd

F32 = mybir.dt.float32
BF16 = mybir.dt.bfloat16
AF = mybir.ActivationFunctionType
ALU = mybir.AluOpType
AX = mybir.AxisListType

D = 1024
S = 2048
NT = 4
NSUB = 16
DEPTH = 2
NH = 8
FF_DENSE = 2816
FF_EXP = 3584
NE = 8
EPS = 1e-6
NSLAB = 3
MASK_NEG = -30000.0
SPARSE_MOE = True


class Op:
    __slots__ = ("eng", "fn", "reads", "writes", "dma", "deps", "signal", "sem", "val", "region")

    def __init__(self, eng, fn, reads, writes, dma):
        self.eng = eng
        self.fn = fn
        self.reads = tuple(reads)
        self.writes = tuple(writes)
        self.dma = dma
        self.deps = []
        self.signal = False
        self.sem = None
        self.val = 0
        self.region = None


class Region:
    def __init__(self, flag_ap, parent=None):
        self.flag_ap = flag_ap
        self.pre = []
        self.parent = parent
        self.depth = 0 if parent is None else parent.depth + 1

    def ancestor_at(self, depth):
        r = self
        while r.depth > depth:
            r = r.parent
        return r


class Prog:
    ENGS = ("pe", "act", "dve", "pool", "sp")

    def __init__(self):
        self.ops = []
        self.cur_region = None

    def op(self, eng, fn, reads=(), writes=(), dma=None):
        o = Op(eng, fn, reads, writes, dma)
        o.region = self.cur_region
        self.ops.append(o)
        return o

    def begin_region(self, flag_ap):
        r = Region(flag_ap, self.cur_region)
        self.ops.append(("region_begin", r))
        self.cur_region = r

    def end_region(self):
        self.cur_region = self.cur_region.parent
        self.ops.append(("barrier_all",))

    def barrier(self, engs=("pe", "act", "dve")):
        self.ops.append(("barrier", tuple(engs)))

    def analyse(self):
        last_w = {}
        readers = {}
        last_dma = {}
        last_on_eng = {}
        pending = {e: [] for e in self.ENGS}
        pending_all = {e: [] for e in self.ENGS}
        real = []
        for o in self.ops:
            if isinstance(o, tuple):
                if o[0] == "barrier":
                    lasts = [last_on_eng[e] for e in o[1] if e in last_on_eng]
                    for e in o[1]:
                        pending[e] = list(lasts)
                else:
                    lasts = [last_on_eng[e] for e in self.ENGS if e in last_on_eng] + list(last_dma.values())
                    for d in lasts:
                        d.signal = True
                    if o[0] == "region_begin":
                        o[1].pre = list(lasts)
                    for e in self.ENGS:
                        pending_all[e] = list(lasts)
                continue
            deps = set()
            for r in o.reads:
                if r in last_w:
                    deps.add(last_w[r])
            for w in o.writes:
                if w in last_w:
                    deps.add(last_w[w])
                for rd in readers.get(w, ()):
                    deps.add(rd)
            if o.dma is not None and o.dma in last_dma:
                deps.add(last_dma[o.dma])
            final = []
            for d in deps:
                if d is o:
                    continue
                if d.dma is None and d.eng == o.eng:
                    if o.eng == "pe":
                        continue
                    if not (set(o.reads) & set(d.writes)):
                        continue
                final.append(d)
            for d in pending[o.eng]:
                if d.eng != o.eng or d.dma is not None:
                    final.append(d)
            pending[o.eng] = []
            final.extend(pending_all[o.eng])
            pending_all[o.eng] = []
            for d in final:
                d.signal = True
            o.deps = final
            for w in o.writes:
                last_w[w] = o
                readers[w] = []
            for r in o.reads:
                readers.setdefault(r, []).append(o)
            if o.dma is not None:
                last_dma[o.dma] = o
            last_on_eng[o.eng] = o
            real.append(o)
        self.real = real

    def emit(self, nc, stack):
        self.analyse()
        eng_sem = {e: stack.enter_context(nc.semaphore("s_" + e)) for e in self.ENGS}
        dma_sem = {}
        cnt = {}
        for o in self.real:
            if not o.signal:
                continue
            if o.dma is not None:
                if o.dma not in dma_sem:
                    dma_sem[o.dma] = stack.enter_context(nc.semaphore("d%d" % len(dma_sem)))
                o.sem = dma_sem[o.dma]
                cnt[o.sem] = cnt.get(o.sem, 0) + 16
            else:
                o.sem = eng_sem[o.eng]
                cnt[o.sem] = cnt.get(o.sem, 0) + 1
            o.val = cnt[o.sem]
        per = {e: [o for o in self.real if o.eng == e] for e in self.ENGS}
        block = stack.enter_context(nc.Block())

        def run(h, ops):
            known = {}

            def waits(deps):
                need = {}
                for d in deps:
                    if known.get(d.sem, 0) < d.val:
                        need[d.sem] = max(need.get(d.sem, 0), d.val)
                for sem, v in need.items():
                    h.wait_ge(sem, v)
                    known[sem] = v

            def emit_op(o):
                waits(o.deps)
                if o.fn is None:
                    return
                ins = o.fn(h)
                if o.signal:
                    ins.then_inc(o.sem, 16 if o.dma is not None else 1)

            def emit_seq(seq, depth, rf):
                i = 0
                while i < len(seq):
                    o = seq[i]
                    od = -1 if o.region is None else o.region.depth
                    if od < depth:
                        emit_op(o)
                        i += 1
                        continue
                    R = o.region.ancestor_at(depth)
                    j = i
                    while j < len(seq) and seq[j].region is not None and seq[j].region.depth >= depth \
                            and seq[j].region.ancestor_at(depth) is R:
                        j += 1
                    body = seq[i:j]
                    waits(R.pre)
                    saved = dict(known)
                    h.reg_load(rf, R.flag_ap)
                    with h.If_ne(rf, 0):
                        emit_seq(body, depth + 1, rf)
                    with h.Else():
                        incs = {}
                        for b in body:
                            if b.signal:
                                incs[b.sem] = incs.get(b.sem, 0) + (16 if b.dma is not None else 1)
                        for sem, v in incs.items():
                            h.sem_inc(sem, v)
                    known.clear()
                    known.update(saved)
                    i = j

            with h.register("rflag") as rf:
                emit_seq(ops, 0, rf)

        @block.tensor
        def _(h):
            run(h, per["pe"])

        @block.scalar
        def _(h):
            run(h, per["act"])

        @block.vector
        def _(h):
            run(h, per["dve"])

        @block.gpsimd
        def _(h):
            run(h, per["pool"])

        @block.sync
        def _(h):
            run(h, per["sp"])


def build_program(layers, debug=None):
    nc = bass.Bass("TRN2", target_bir_lowering=False)
    P = Prog()
    stack = ExitStack()

    def din(name, shape, dt=F32):
        return nc.dram_tensor(name, list(shape), dt, kind="ExternalInput").ap()

    x_in = din("x", [S, D])
    cT_in = din("cT", [128, 8])
    w_ada = din("w_ada", [DEPTH, D, 6 * D])
    b_ada = din("b_ada", [DEPTH, 6 * D])
    gmix_in = din("gmix", [DEPTH, 128, 8])
    gffn_in = din("gffn", [DEPTH, 128, 8])
    w_in = din("w_in", [DEPTH, D, 2560])
    w_out = din("w_out", [DEPTH, D, D])
    convw_in = din("convw", [DEPTH, 128, 4, 31])
    convp_in = din("convp", [DEPTH, 128, 3, 4])
    qkg_in = din("qkg", [DEPTH, 128, 2])
    bias2_in = din("bias2", [DEPTH, 128, NH, 640])
    if 0 in layers:
        ffn_wg = din("ffn_wg", [1, D, FF_DENSE])
        ffn_wu = din("ffn_wu", [1, D, FF_DENSE])
        ffn_wd = din("ffn_wd", [1, FF_DENSE, D])
    wr_in = din("wr", [1, 128, 8, NE])
    br_in = din("br", [1, 128, NE])
    if 1 in layers:
        moe_wgu = din("moe_wgu", [1, NE, D, 2 * FF_EXP])
        moe_wd = din("moe_wd", [1, NE, FF_EXP, D])
    if 0 not in layers:
        ffn_wg = ffn_wu = ffn_wd = None
    y_out = nc.dram_tensor("y", [S, D], F32, kind="ExternalOutput").ap()
    dbg_out = {}
    if debug:
        for name, shape in debug.items():
            dbg_out[name] = nc.dram_tensor("dbg_" + name, list(shape), F32, kind="ExternalOutput").ap()

    def sb(name, shape, dt):
        return stack.enter_context(nc.sbuf_tensor("sb_" + name, list(shape), dt))

    x_tok = sb("x_tok", [128, NSUB, D], F32)
    kvact = sb("kvact", [128, 16384], BF16)
    kT = kvact[:, 0:8192].rearrange("p (c t) -> p c t", c=4)
    v_tok = kvact[:, 8192:16384].rearrange("p (s d) -> p s d", s=16)
    act = kvact[:, 0:28 * 512].rearrange("p (j t) -> p j t", j=28)
    hT = sb("hT", [128, 8, 512], BF16)
    qT = sb("qT", [128, 4, 512], BF16)
    zb = sb("zb", [128, 4, 542], BF16)
    ycat = sb("ycat", [128, 8, 512], BF16)
    slabs = [sb("slab%d" % i, [128, 8, 512], BF16) for i in range(NSLAB)]
    bias2 = sb("bias2", [128, NH, 640], BF16)
    gbc = sb("gbc", [128, 2, D], F32)
    arena = sb("arena", [128, 7680], F32)
    ident_f = sb("ident_f", [128, 128], F32)
    ident_b = sb("ident_b", [128, 128], BF16)
    ones_f = sb("ones_f", [128, 128], F32)
    bdiag_f = sb("bdiag_f", [128, 128], F32)
    cT = sb("cT", [128, 8], F32)
    cact = sb("cact", [128, 8], BF16)
    modcols = [sb("modcol%d" % i, [128, 4, 8], F32) for i in range(2)]
    LCUR = [0]
    gcol = sb("gcol", [128, 2, 8], F32)
    ABcols = [sb("ABcol%d" % i, [128, 2, 8], F32) for i in range(2)]
    convw = sb("convw", [128, 4, 31], F32)
    convp = sb("convp", [128, 3, 4], F32)
    qkg = sb("qkg", [128, 2], F32)
    wr = sb("wr", [128, 8, NE], F32)
    brb = sb("brb", [128, NE], F32)
    comb = sb("comb", [128, NSUB, NE], F32)
    rowb = sb("rowb", [1, 2, 512], F32)
    brow = sb("brow", [1, 2, 512], F32)
    small = sb("small", [128, 96], F32)
    one_f = ones_f[0:1, 0:1]
    epsc = sb("epsc", [128, 1], F32)

    psd = [stack.enter_context(nc.psum_tensor("psd%d" % i, [128, 1024], F32)) for i in range(4)]
    ps = [psd[i // 2][:, (i % 2) * 512:(i % 2) * 512 + 512] for i in range(8)]

    misc_i = [0]

    def dma_sp(out, in_, writes, reads=()):
        lane = ("misc", misc_i[0] % 4)
        misc_i[0] += 1
        P.op("sp", lambda h: h.dma_start(out=out, in_=in_), reads=reads, writes=writes, dma=lane)

    slab_i = [0]

    def load_slab(src_ap, kparts, ncols):
        i = slab_i[0] % NSLAB
        slab_i[0] += 1
        t = slabs[i]
        key = ("slab", i)
        P.op("pool", lambda h: h.dma_start(out=t[:, 0:kparts, 0:ncols], in_=src_ap),
             writes=[key], dma=key)
        return t, key

    def mm(out, lhsT, rhs, start, stop, reads, writes):
        P.op("pe", lambda h: h.matmul(out, lhsT, rhs, start=start, stop=stop), reads=reads, writes=writes)

    def dve(fn, reads, writes):
        P.op("dve", fn, reads=reads, writes=writes)

    def actop(fn, reads, writes):
        P.op("act", fn, reads=reads, writes=writes)

    def A(off, n):
        return arena[:, off:off + n]

    def AB(off_f32, n_bf16):
        return arena[:, off_f32:off_f32 + n_bf16 // 2].bitcast(BF16)

    dve(lambda h: h.memset(ones_f[:], 1.0), [], ["ones_f"])
    dve(lambda h: h.memset(epsc[:], EPS), [], ["epsc"])
    dve(lambda h: h.memset(bdiag_f[:], 0.0), [], ["bdiag_f"])
    dve(lambda h: h.memset(bdiag_f[0:64, 0:64], 1.0), [], ["bdiag_f"])
    dve(lambda h: h.memset(bdiag_f[64:128, 64:128], 1.0), [], ["bdiag_f"])
    dve(lambda h: h.memset(zb[:, :, 0:30], 0.0), [], ["zb"])
    ident_in = din("ident", [128, 128])
    I32 = mybir.dt.int32
    ustrict = sb("ustrict", [128, 128], F32)
    iota_f = sb("iota_f", [128, 512], F32)
    ustrict_in = din("ustrict", [128, 128])
    iota_in = din("iota", [128, 512])
    dma_sp(ustrict[:], ustrict_in[:, :], ["ustrict"])
    dma_sp(iota_f[:], iota_in[:, :], ["iota_f"])
    dma_sp(ident_f[:], ident_in[:, :], ["ident_f"])
    dve(lambda h: h.tensor_copy(out=ident_b[:], in_=ident_f[:]), ["ident_f"], ["ident_b"])
    dma_sp(cT[:], cT_in[:, :], ["cT"])
    actop(lambda h: h.activation(out=cact[:], in_=cT[:], func=AF.Silu), ["cT"], ["cact"])
    xv = x_in.rearrange("(s p) d -> p s d", p=128)
    for s4 in range(4):
        P.op("sp", lambda h, s4=s4: h.dma_start(out=x_tok[:, 4 * s4:4 * s4 + 4, :], in_=xv[:, 4 * s4:4 * s4 + 4, :]),
             writes=[("x", 4 * s4 + i) for i in range(4)], dma=("xio", s4))
    P.barrier()

    def layer_consts(l):
        dma_sp(gcol[:, 0, :], gmix_in[l], ["gcol"])
        dma_sp(gcol[:, 1, :], gffn_in[l], ["gcol"])
        dma_sp(convw[:], convw_in[l], ["convw"])
        dma_sp(convp[:], convp_in[l], ["convp"])
        dma_sp(qkg[:], qkg_in[l], ["qkg"])
        P.op("pool", lambda h: h.dma_start(out=bias2[:], in_=bias2_in[l]), writes=["bias2"], dma=("b2", 0))
        if l % 2 == 1:
            dma_sp(wr[:], wr_in[l // 2], ["wr"])
            dma_sp(brb[:], br_in[l // 2], ["brb"])

    def mod_slab(l, sl):
        modcol = modcols[l % 2]
        mk = ("modcol", l % 2)
        wv = w_ada[l].rearrange("(k p) f -> p k f", p=128)
        colps = ps[7]
        t, key = load_slab(wv[:, :, sl * 512:(sl + 1) * 512], 8, 512)
        rb = sl % 2
        dma_sp(brow[0:1, rb, :], b_ada[l:l + 1, sl * 512:(sl + 1) * 512], [("brow", rb)])
        pr = ps[sl % 2]
        for k in range(8):
            mm(pr[0:1, :], cact[:, k:k + 1], t[:, k, :], k == 0, k == 7, [key, "cact"], [("ps", sl % 2)])
        dve(lambda h: h.tensor_tensor(out=rowb[0:1, rb, :], in0=pr[0:1, :], in1=brow[0:1, rb, :], op=ALU.add),
            [("ps", sl % 2), ("brow", rb)], [("rowb", rb)])
        v = sl // 2
        half = sl % 2
        if v in (2, 5):
            gi = 0 if v == 2 else 1
            pb = ps[2 + sl % 2]
            mm(pb[:, :], ones_f[0:1, :], rowb[0:1, rb, :], True, True, [("rowb", rb), "ones_f"], [("ps", 2 + sl % 2)])
            actop(lambda h: h.activation(out=gbc[:, gi, half * 512:(half + 1) * 512], in_=pb[:, :], func=AF.Identity),
                  [("ps", 2 + sl % 2)], [("gbc", gi)])
        else:
            vi = {0: 0, 1: 1, 3: 2, 4: 3}[v]
            for cc in range(4):
                mm(colps[:, cc:cc + 1], rowb[0:1, rb, cc * 128:(cc + 1) * 128], one_f, True, True,
                   [("rowb", rb), "ones_f"], [("ps", 7)])
            dve(lambda h: h.tensor_copy(out=modcol[:, vi, half * 4:(half + 1) * 4], in_=colps[:, 0:4]), [("ps", 7)], [mk])

    def mod_finish(l, parts=(0, 1)):
        modcol = modcols[l % 2]
        ABcol = ABcols[l % 2]
        for i, sci in ((0, 1), (1, 3)):
            if i not in parts:
                continue
            dve(lambda h, i=i, sci=sci: h.scalar_tensor_tensor(out=ABcol[:, i, :], in0=modcol[:, sci, :], scalar=1.0, in1=gcol[:, i, :],
                                                             op0=ALU.add, op1=ALU.mult), [("modcol", l % 2), "gcol"], [("ABcol", l % 2)])

    def norm_tile(t, which, router, dst=None):
        shi = 0 if which == 0 else 2
        modcol = modcols[LCUR[0] % 2]
        ABcol = ABcols[LCUR[0] % 2]
        mkeys = [("ABcol", LCUR[0] % 2), ("modcol", LCUR[0] % 2)]
        for ss_ in range(4):
            s = 4 * t + ss_
            if dst is None:
                dT, doff, dkey = hT, ss_ * 128, ("hT", ss_)
            else:
                dT, doff, dkey = dst, s * 128, ("h2", s)
            xb = ss_ % 2
            xn = A(xb * 1024, 1024)
            st = small[:, xb:xb + 1]
            st2 = small[:, 2 + xb:3 + xb]
            actop(lambda h, xn=xn, s=s, st=st: h.activation(out=xn, in_=x_tok[:, s, :], func=AF.Square, accum_out=st),
                  [("x", s)], [("xn", xb), ("st", xb)])
            actop(lambda h, st=st, st2=st2: h.activation(out=st2, in_=st, func=AF.Sqrt, scale=1.0 / D, bias=epsc[:, 0:1]),
                  [("st", xb)], [("st2", xb)])
            dve(lambda h, st2=st2: h.reciprocal(out=st2, in_=st2), [("st2", xb)], [("st2", xb)])
            actop(lambda h, xn=xn, s=s, st2=st2: h.activation(out=xn, in_=x_tok[:, s, :], func=AF.Identity, scale=st2),
                  [("x", s), ("st2", xb)], [("xn", xb)])
            h2f = A(2048, 1024).rearrange("p (c t) -> p c t", c=8)
            for hb in range(2):
                pb = ps[(2 * ss_ + hb) % 4]
                pk = ("ps", (2 * ss_ + hb) % 4)
                for cc in range(4):
                    c = hb * 4 + cc
                    mm(pb[:, cc * 128:(cc + 1) * 128], xn[:, c * 128:(c + 1) * 128], ident_f[:], True, True, [("xn", xb)], [pk])
                for cc in range(4):
                    c = hb * 4 + cc
                    if router:
                        actop(lambda h, pb=pb, cc=cc, c=c: h.activation(out=h2f[:, c, :], in_=pb[:, cc * 128:(cc + 1) * 128], func=AF.Identity,
                                                                       scale=ABcol[:, which, c:c + 1], bias=modcol[:, shi, c:c + 1]),
                              [pk] + mkeys, [("h2f", c)])
                    else:
                        actop(lambda h, pb=pb, cc=cc, c=c, dT=dT, doff=doff: h.activation(out=dT[:, c, doff:doff + 128], in_=pb[:, cc * 128:(cc + 1) * 128],
                                                                               func=AF.Identity, scale=ABcol[:, which, c:c + 1], bias=modcol[:, shi, c:c + 1]),
                              [pk] + mkeys, [dkey])
            if router:
                xnb = kvact[:, 0:16384].rearrange("p (s d) -> p s d", s=16)
                dve(lambda h, xn=xn, s=s: h.tensor_copy(out=xnb[:, s, :], in_=xn), [("xn", xb)], [("xnb", s)])
                route_subtile(s)

    def route_subtile(s):
        h2f = A(2048, 1024).rearrange("p (c t) -> p c t", c=8)
        pl = ps[4 + s % 2]
        pk = ("ps", 4 + s % 2)
        for c in range(8):
            mm(pl[:, 0:NE], h2f[:, c, :], wr[:, c, :], c == 0, c == 7, [("h2f", c), "wr"], [pk])
        lg = small[:, 8:16]
        m1 = small[:, 16:17]
        m2 = small[:, 17:18]
        nm1 = small[:, 18:19]
        den = small[:, 19:20]
        l2 = small[:, 28:36]
        ex = small[:, 36:44]
        sel = small[:, 44:52]
        dve(lambda h: h.tensor_tensor(out=lg, in0=pl[:, 0:NE], in1=brb[:], op=ALU.add), [pk, "brb"], ["lg"])
        dve(lambda h: h.tensor_reduce(out=m1, in_=lg, axis=AX.X, op=ALU.max), ["lg"], ["m1"])
        dve(lambda h: h.tensor_scalar(out=l2, in0=lg, scalar1=m1, scalar2=-1e30, op0=ALU.is_equal, op1=ALU.mult), ["lg", "m1"], ["l2"])
        dve(lambda h: h.tensor_tensor(out=l2, in0=l2, in1=lg, op=ALU.add), ["l2", "lg"], ["l2"])
        dve(lambda h: h.tensor_reduce(out=m2, in_=l2, axis=AX.X, op=ALU.max), ["l2"], ["m2"])
        dve(lambda h: h.tensor_scalar(out=nm1, in0=m1, scalar1=-1.0, scalar2=None, op0=ALU.mult), ["m1"], ["nm1"])
        actop(lambda h: h.activation(out=ex, in_=lg, func=AF.Exp, bias=nm1), ["lg", "nm1"], ["ex"])
        dve(lambda h: h.scalar_tensor_tensor(out=sel, in0=lg, scalar=m2, in1=ex, op0=ALU.is_ge, op1=ALU.mult), ["lg", "m2", "ex"], ["sel"])
        dve(lambda h: h.tensor_reduce(out=den, in_=sel, axis=AX.X, op=ALU.add), ["sel"], ["den"])
        dve(lambda h: h.reciprocal(out=den, in_=den), ["den"], ["den"])
        dve(lambda h: h.tensor_scalar(out=comb[:, s, :], in0=sel, scalar1=den, scalar2=None, op0=ALU.mult), ["sel", "den"], [("comb", s)])

    def proj_tile(l, t):
        wv = w_in[l].rearrange("(k p) f -> p k f", p=128)
        sa, ka = load_slab(wv[:, :, 0:512], 8, 512)
        sg_, kg_ = load_slab(wv[:, :, 512:1024], 8, 512)
        for c in range(4):
            pa, pka = ps[c % 2], ("ps", c % 2)
            pg, pkg = ps[2 + c % 2], ("ps", 2 + c % 2)
            for k in range(8):
                mm(pa[:, :], sa[:, k, c * 128:(c + 1) * 128], hT[:, k, :], k == 0, k == 7, [ka] + [("hT", i) for i in range(4)], [pka])
            for k in range(8):
                mm(pg[:, :], sg_[:, k, c * 128:(c + 1) * 128], hT[:, k, :], k == 0, k == 7, [kg_] + [("hT", i) for i in range(4)], [pkg])
            sgt = A(3072 + (c % 2) * 512, 512)
            actop(lambda h, pg=pg, sgt=sgt: h.activation(out=sgt, in_=pg[:, :], func=AF.Sigmoid), [pkg], [("sgt", c % 2)])
            dve(lambda h, pa=pa, sgt=sgt, c=c: h.tensor_tensor(out=zb[:, c, 30:542], in0=pa[:, :], in1=sgt, op=ALU.mult),
                [pka, ("sgt", c % 2)], [("zb", c)])
        for qi in range(2):
            sw, kw = load_slab(wv[:, :, 1024 + qi * 512:1536 + qi * 512], 8, 512)
            sqs = [A(3072 + c * 512, 512) for c in range(4)]
            for c in range(4):
                for k in range(8):
                    mm(ps[c][:, :], sw[:, k, c * 128:(c + 1) * 128], hT[:, k, :], k == 0, k == 7, [kw] + [("hT", i) for i in range(4)], [("ps", c)])
            for c in range(4):
                actop(lambda h, c=c: h.activation(out=sqs[c], in_=ps[c][:, :], func=AF.Square), [("ps", c)], [("sgt", c)])
            for c in range(4):
                mm(ps[4 + c][:, :], bdiag_f[:], sqs[c], True, True, [("sgt", c)], [("ps", 4 + c)])
            for c in range(4):
                actop(lambda h, c=c: h.activation(out=sqs[c], in_=ps[4 + c][:, :], func=AF.Sqrt, scale=1.0 / 64, bias=epsc[:, 0:1]),
                      [("ps", 4 + c)], [("sgt", c)])
            for c in range(4):
                dve(lambda h, c=c: h.reciprocal(out=sqs[c], in_=sqs[c]), [("sgt", c)], [("sgt", c)])
            for c in range(4):
                if qi == 0:
                    dst = qT[:, c, :]
                    dk = ("qT", c)
                else:
                    dst = kT[:, c, t * 512:(t + 1) * 512]
                    dk = ("kT", c, t)
                dve(lambda h, c=c, dst=dst, qi=qi: h.scalar_tensor_tensor(out=dst, in0=ps[c][:, :], scalar=small[:, 64 + qi:65 + qi], in1=sqs[c],
                                                                        op0=ALU.mult, op1=ALU.mult),
                    [("ps", c), ("sgt", c), "qkg8"], [dk])
        sv, kv = load_slab(wv[:, :, 2048:2560], 8, 512)
        for ss_ in range(4):
            pv, pkv = ps[ss_ % 2], ("ps", ss_ % 2)
            for k in range(8):
                mm(pv[:, :], hT[:, k, ss_ * 128:(ss_ + 1) * 128], sv[:, k, :], k == 0, k == 7, [kv, ("hT", ss_)], [pkv])
            actop(lambda h, pv=pv, ss_=ss_: h.activation(out=v_tok[:, 4 * t + ss_, :], in_=pv[:, :], func=AF.Identity), [pkv], [("v", 4 * t + ss_)])

    def conv_tile(l, t):
        cv = A(0, 2048).rearrange("p (c t) -> p c t", c=4)
        diag = AB(2048, 31 * 128).rearrange("p (a b) -> p a b", a=31)
        sqb = [A(4096, 512), A(4608, 512)]
        t1b = [A(5120, 512), A(5632, 512)]
        mean = A(6144, 512)
        rstd = A(6656, 512)
        msq = A(7168, 512)
        diag2 = flat_of(hT)[:, 0:31 * 128].rearrange("p (a b) -> p a b", a=31)
        hkeys = [("hT", i) for i in range(4)]
        for c in range(4):
            dg = diag if c % 2 == 0 else diag2
            dkeys = [("diag", c % 2)] + (hkeys if c % 2 == 1 else [])
            dve(lambda h, c=c, dg=dg: h.tensor_tensor(out=dg, in0=ident_b[:].unsqueeze(1).to_broadcast([128, 31, 128]),
                                                      in1=convw[:, c, :].unsqueeze(2).to_broadcast([128, 31, 128]), op=ALU.mult),
                ["convw"], dkeys)
            pc, pkc = ps[c], ("ps", c)
            for tap in range(31):
                mm(pc[:, :], dg[:, tap, :], zb[:, c, tap:tap + 512], tap == 0, tap == 30, dkeys + [("zb", c)], [pkc])
            actop(lambda h, pc=pc, c=c: h.activation(out=cv[:, c, :], in_=pc[:, :], func=AF.Identity, bias=convp[:, 0, c:c + 1]),
                  [pkc, "convp"], [("cv", c)])
            sq = sqb[c % 2]
            actop(lambda h, sq=sq, c=c: h.activation(out=sq, in_=cv[:, c, :], func=AF.Square), [("cv", c)], [("sq", c % 2)])
            mm(ps[4][:, :], ones_f[:], cv[:, c, :], c == 0, c == 3, [("cv", c)], [("ps", 4)])
            mm(ps[5][:, :], ones_f[:], sq, c == 0, c == 3, [("sq", c % 2)], [("ps", 5)])
        dve(lambda h: h.tensor_copy(out=zb[:, :, 0:30], in_=zb[:, :, 512:542]), [("zb", c) for c in range(4)], [("zb", c) for c in range(4)])
        dve(lambda h: h.tensor_scalar(out=mean, in0=ps[4][:, :], scalar1=1.0 / 512, scalar2=None, op0=ALU.mult), [("ps", 4)], ["mean"])
        dve(lambda h: h.tensor_tensor(out=msq, in0=mean, in1=mean, op=ALU.mult), ["mean"], ["msq"])
        dve(lambda h: h.scalar_tensor_tensor(out=rstd, in0=ps[5][:, :], scalar=1.0 / 512, in1=msq, op0=ALU.mult, op1=ALU.subtract),
            [("ps", 5), "msq"], ["rstd"])
        actop(lambda h: h.activation(out=rstd, in_=rstd, func=AF.Sqrt, bias=epsc[:, 0:1]), ["rstd"], ["rstd"])
        dve(lambda h: h.reciprocal(out=rstd, in_=rstd), ["rstd"], ["rstd"])
        for c in range(4):
            t1 = t1b[c % 2]
            dve(lambda h, t1=t1, c=c: h.tensor_tensor(out=t1, in0=cv[:, c, :], in1=mean, op=ALU.subtract), [("cv", c), "mean"], [("t1", c % 2)])
            dve(lambda h, t1=t1: h.tensor_tensor(out=t1, in0=t1, in1=rstd, op=ALU.mult), [("t1", c % 2), "rstd"], [("t1", c % 2)])
            actop(lambda h, t1=t1, c=c: h.activation(out=ycat[:, c, :], in_=t1, func=AF.Silu, scale=convp[:, 1, c:c + 1], bias=convp[:, 2, c:c + 1]),
                  [("t1", c % 2), "convp"], [("ycat", c)])

    def attn_tile(l, t):
        pb_ = [AB(5120, 640), AB(5440, 640)]
        pTb = [AB(5760, 640), AB(6080, 640)]
        yatt_sb = AB(6400, 512)
        rs_alls = [small[:, 66:74], small[:, 82:90]]
        rinv = small[:, 20:28]
        pTt = psd[2]
        kPT = [("ps", 4), ("ps", 5)]
        blocks = []
        for pi in range(4):
            i = 4 * t + pi
            c0 = max(0, 2 * i - 8)
            k0 = c0 * 64
            nk = (2 * i + 2 - c0) * 64
            for hh in range(NH):
                blocks.append(dict(pi=pi, hh=hh, k0=k0, nk=nk, boff=640 - nk, q0=pi * 128, nkc=nk // 128))

        def stage_A(b):
            B_ = blocks[b]
            pi, hh, k0, nk, boff, q0 = B_["pi"], B_["hh"], B_["k0"], B_["nk"], B_["boff"], B_["q0"]
            c, hp = hh // 2, (hh % 2) * 64
            bb = b % 2
            pS = psd[bb]
            kS = [("ps", 2 * bb), ("ps", 2 * bb + 1)]
            n0 = min(nk, 512)
            kreads = [("kT", c, tt) for tt in range(k0 // 512, (k0 + nk - 1) // 512 + 1)]
            mm(pS[:, 0:n0], qT[hp:hp + 64, c, q0:q0 + 128], kT[hp:hp + 64, c, k0:k0 + n0], True, False, [("qT", c)] + kreads, kS)
            mm(pS[:, 0:n0], ident_b[:], bias2[:, hh, boff:boff + n0], False, True, ["bias2"], kS)
            if nk > 512:
                mm(pS[:, 512:nk], qT[hp:hp + 64, c, q0:q0 + 128], kT[hp:hp + 64, c, k0 + 512:k0 + nk], True, False, [("qT", c)] + kreads, kS)
                mm(pS[:, 512:nk], ident_b[:], bias2[:, hh, boff + 512:boff + nk], False, True, ["bias2"], kS)
            mx = small[:, 74 + bb:75 + bb]
            dve(lambda h: h.tensor_reduce(out=mx, in_=pS[:, 0:nk], axis=AX.X, op=ALU.max, negate=True), kS, [("mx", bb)])
            pp = pb_[bb]
            rs = rs_alls[pi % 2]
            actop(lambda h: h.activation(out=pp[:, 0:nk], in_=pS[:, 0:nk], func=AF.Exp, bias=mx, accum_out=rs[:, hh:hh + 1]),
                  kS + [("mx", bb)], [("p", bb), ("rs_all", pi % 2, hh)])

        def stage_B(b):
            B_ = blocks[b]
            nk, nkc = B_["nk"], B_["nkc"]
            bb = b % 2
            pp = pb_[bb]
            for kc in range(nkc):
                mm(pTt[:, kc * 128:(kc + 1) * 128], pp[:, kc * 128:(kc + 1) * 128], ident_b[:], True, True, [("p", bb)], kPT)
            pT = pTb[bb]
            if b % 2 == 0:
                actop(lambda h: h.activation(out=pT[:, 0:nk], in_=pTt[:, 0:nk], func=AF.Identity), kPT, [("pT", bb)])
            else:
                dve(lambda h: h.tensor_copy(out=pT[:, 0:nk], in_=pTt[:, 0:nk]), kPT, [("pT", bb)])

        def stage_C(b):
            B_ = blocks[b]
            pi, hh, k0, nkc = B_["pi"], B_["hh"], B_["k0"], B_["nkc"]
            bb = b % 2
            pT = pTb[bb]
            yb = 6 + pi % 2
            for kc in range(nkc):
                mm(ps[yb][:, hh * 64:(hh + 1) * 64], pT[:, kc * 128:(kc + 1) * 128], v_tok[:, k0 // 128 + kc, hh * 64:(hh + 1) * 64],
                   kc == 0, kc == nkc - 1, [("pT", bb), ("v", k0 // 128 + kc)], [("ps", yb)])

        def finalize(pi):
            q0 = pi * 128
            yb = 6 + pi % 2
            rs = rs_alls[pi % 2]
            dve(lambda h: h.reciprocal(out=rinv, in_=rs), [("rs_all", pi % 2, hh) for hh in range(NH)], ["rinv"])
            dve(lambda h: h.tensor_tensor(out=yatt_sb.rearrange("p (a b) -> p a b", a=NH), in0=ps[yb][:, :].rearrange("p (a b) -> p a b", a=NH),
                                          in1=rinv.unsqueeze(2).to_broadcast([128, NH, 64]), op=ALU.mult), [("ps", yb), "rinv"], ["yatt_sb"])
            for cc in range(4):
                mm(pTt[:, cc * 128:(cc + 1) * 128], yatt_sb[:, cc * 128:(cc + 1) * 128], ident_b[:], True, True, ["yatt_sb"], kPT)
            actop(lambda h: h.activation(out=ycat[:, 4:8, q0:q0 + 128], in_=pTt[:, 0:512].rearrange("p (a b) -> p a b", a=4), func=AF.Identity),
                  kPT, [("ycat", 4 + cc) for cc in range(4)])

        nb = len(blocks)
        stage_A(0)
        stage_A(1)
        for b in range(nb):
            stage_B(b)
            if b + 2 < nb:
                stage_A(b + 2)
            stage_C(b)
            if blocks[b]["hh"] == NH - 1:
                finalize(blocks[b]["pi"])

    def outproj_tile(l, t):
        wv = w_out[l].rearrange("(k p) f -> p k f", p=128)
        tmpb = [A(6656, 512), A(7168, 512)]
        n = 0
        for half in range(2):
            sw, kw = load_slab(wv[:, :, half * 512:(half + 1) * 512], 8, 512)
            for ss_ in range(4):
                s = 4 * t + ss_
                po, pko = ps[n % 2], ("ps", n % 2)
                tb = tmpb[n % 2]
                for k in range(8):
                    mm(po[:, :], ycat[:, k, ss_ * 128:(ss_ + 1) * 128], sw[:, k, :], k == 0, k == 7, [kw, ("ycat", k)], [pko])
                dve(lambda h, po=po, tb=tb, half=half: h.tensor_tensor(out=tb, in0=po[:, :], in1=gbc[:, 0, half * 512:(half + 1) * 512], op=ALU.mult),
                    [pko, ("gbc", 0)], [("tmp", n % 2)])
                dve(lambda h, tb=tb, s=s, half=half: h.tensor_tensor(out=x_tok[:, s, half * 512:(half + 1) * 512], in0=x_tok[:, s, half * 512:(half + 1) * 512], in1=tb, op=ALU.add),
                    [("tmp", n % 2), ("x", s)], [("x", s)])
                n += 1

    def ffn_item(t, wg, wu, wd, nj, e):
        wgv = wg.rearrange("(k p) f -> p k f", p=128)
        wuv = wu.rearrange("(k p) f -> p k f", p=128)
        wdv = wd.rearrange("(j p) d -> p j d", p=128)
        sgb = [A(0, 512), A(512, 512)]
        tmpb = [A(1024, 512), A(1536, 512)]
        hreads = [("hT", i) for i in range(4)]
        nslab = (nj + 3) // 4
        for sl in range(nslab):
            j0 = sl * 4
            njs = min(4, nj - j0)
            sgw, kgw = load_slab(wgv[:, :, j0 * 128:(j0 + njs) * 128], 8, njs * 128)
            suw, kuw = load_slab(wuv[:, :, j0 * 128:(j0 + njs) * 128], 8, njs * 128)
            for jj in range(njs):
                j = j0 + jj
                pg, pkg = ps[j % 2], ("ps", j % 2)
                pu, pku = ps[2 + j % 2], ("ps", 2 + j % 2)
                for k in range(8):
                    mm(pg[:, :], sgw[:, k, jj * 128:(jj + 1) * 128], hT[:, k, :], k == 0, k == 7, [kgw] + hreads, [pkg])
                for k in range(8):
                    mm(pu[:, :], suw[:, k, jj * 128:(jj + 1) * 128], hT[:, k, :], k == 0, k == 7, [kuw] + hreads, [pku])
                sg = sgb[j % 2]
                actop(lambda h, sg=sg, pg=pg: h.activation(out=sg, in_=pg[:, :], func=AF.Silu), [pkg], [("sg", j % 2)])
                dve(lambda h, sg=sg, pu=pu, j=j: h.tensor_tensor(out=act[:, j, :], in0=pu[:, :], in1=sg, op=ALU.mult), [pku, ("sg", j % 2)], [("act", j)])
        nds = (nj + 7) // 8
        n = 0
        for half in range(2):
            for sl in range(nds):
                j0 = sl * 8
                njs = min(8, nj - j0)
                sdw, kdw = load_slab(wdv[:, j0:j0 + njs, half * 512:(half + 1) * 512], njs, 512)
                for jj in range(njs):
                    j = j0 + jj
                    for ss_ in range(4):
                        mm(ps[4 + ss_][:, :], act[:, j, ss_ * 128:(ss_ + 1) * 128], sdw[:, jj, :], j == 0, j == nj - 1, [kdw, ("act", j)], [("ps", 4 + ss_)])
            for ss_ in range(4):
                s = 4 * t + ss_
                tb = tmpb[n % 2]
                po = ps[4 + ss_]
                if e is None:
                    dve(lambda h, po=po, tb=tb, half=half: h.tensor_tensor(out=tb, in0=po[:, :], in1=gbc[:, 1, half * 512:(half + 1) * 512], op=ALU.mult),
                        [("ps", 4 + ss_), ("gbc", 1)], [("tmp", n % 2)])
                else:
                    dve(lambda h, po=po, tb=tb, half=half, s=s, e=e: h.scalar_tensor_tensor(out=tb, in0=po[:, :], scalar=comb[:, s, e:e + 1], in1=gbc[:, 1, half * 512:(half + 1) * 512],
                                                                                    op0=ALU.mult, op1=ALU.mult),
                        [("ps", 4 + ss_), ("gbc", 1), ("comb", s)], [("tmp", n % 2)])
                dve(lambda h, tb=tb, s=s, half=half: h.tensor_tensor(out=x_tok[:, s, half * 512:(half + 1) * 512], in0=x_tok[:, s, half * 512:(half + 1) * 512], in1=tb, op=ALU.add),
                    [("tmp", n % 2), ("x", s)], [("x", s)])
                n += 1

    def flat_of(tn):
        return tn[:].rearrange("p a b -> p (a b)")
    ffn_ring = [(flat_of(slabs[i]), ("slab", i)) for i in range(NSLAB)]
    ffn_ring.append((arena[:, 3072:5120].bitcast(BF16), ("slab", NSLAB)))
    ffn_ring.append((arena[:, 5120:7168].bitcast(BF16), ("slab", NSLAB + 1)))
    ffn_i = [0]

    def load_ffn(src_ap, down, n0, n1):
        flat, key = ffn_ring[ffn_i[0] % len(ffn_ring)]
        ffn_i[0] += 1
        if down:
            view = flat.rearrange("p (j d) -> p j d", j=4)
        else:
            view = flat.rearrange("p (k f) -> p k f", k=8)
        rd = ["ffn_go"] if key[1] >= NSLAB else []
        P.op("pool", lambda h: h.dma_start(out=view[:, 0:n0, 0:n1], in_=src_ap), reads=rd, writes=[key], dma=key)
        return view, key

    def ffn_experts(wts, nj, hook=None):
        h2all = kvact[:, 0:16384].rearrange("p (c t) -> p c t", c=8)
        actv = [flat_of(hT).rearrange("p (j t) -> p j t", j=2), flat_of(ycat).rearrange("p (j t) -> p j t", j=2)]
        sgb = [A(0, 512), A(512, 512)]
        tmpb = [A(1024, 512), A(1536, 512)]
        n = 0
        m = 0
        for (wg, wu, wd, e) in wts:
            wgv = wg.rearrange("(k p) f -> p k f", p=128)
            wuv = wu.rearrange("(k p) f -> p k f", p=128)
            wdv = wd.rearrange("(j p) d -> p j d", p=128)
            for sl in range((nj + 3) // 4):
                j0 = sl * 4
                njs = min(4, nj - j0)
                if hook is not None:
                    hook()
                sgw, kgw = load_ffn(wgv[:, :, j0 * 128:(j0 + njs) * 128], False, 8, njs * 128)
                suw, kuw = load_ffn(wuv[:, :, j0 * 128:(j0 + njs) * 128], False, 8, njs * 128)
                sdw, kdw = load_ffn(wdv[:, j0:j0 + njs, :], True, njs, 1024)
                dve(lambda h, sdw=sdw, njs=njs: h.tensor_tensor(out=sdw[:, 0:njs, :], in0=sdw[:, 0:njs, :],
                                                                in1=gbc[:, 1, :].unsqueeze(1).to_broadcast([128, njs, 1024]), op=ALU.mult),
                    [kdw, ("gbc", 1)], [kdw])
                for t in range(NT):
                    hreads = [("h2", 4 * t + i) for i in range(4)]
                    for jj in range(njs):
                        pg, pkg = ps[n % 2], ("ps", n % 2)
                        pu, pku = ps[2 + n % 2], ("ps", 2 + n % 2)
                        sg = sgb[n % 2]
                        for k in range(8):
                            mm(pg[:, :], sgw[:, k, jj * 128:(jj + 1) * 128], h2all[:, k, t * 512:(t + 1) * 512], k == 0, k == 7, [kgw] + hreads, [pkg])
                        for k in range(8):
                            mm(pu[:, :], suw[:, k, jj * 128:(jj + 1) * 128], h2all[:, k, t * 512:(t + 1) * 512], k == 0, k == 7, [kuw] + hreads, [pku])
                        actop(lambda h, sg=sg, pg=pg: h.activation(out=sg, in_=pg[:, :], func=AF.Silu), [pkg], [("sg", n % 2)])
                        dst = actv[jj // 2][:, jj % 2, t * 512:(t + 1) * 512]
                        dve(lambda h, sg=sg, pu=pu, dst=dst: h.tensor_tensor(out=dst, in0=pu[:, :], in1=sg, op=ALU.mult), [pku, ("sg", n % 2)], [("act", jj, t)])
                        n += 1
                for s in range(NSUB):
                    for half in range(2):
                        po, pko = ps[4 + m % 4], ("ps", 4 + m % 4)
                        tb = tmpb[m % 2]
                        for jj in range(njs):
                            mm(po[:, :], actv[jj // 2][:, jj % 2, s * 128:(s + 1) * 128], sdw[:, jj, half * 512:(half + 1) * 512], jj == 0, jj == njs - 1,
                               [kdw, ("act", jj, s // 4)], [pko])
                        sc = 1.0 if e is None else comb[:, s, e:e + 1]
                        dve(lambda h, po=po, half=half, s=s, sc=sc: h.scalar_tensor_tensor(out=x_tok[:, s, half * 512:(half + 1) * 512], in0=po[:, :], scalar=sc,
                                                                                       in1=x_tok[:, s, half * 512:(half + 1) * 512], op0=ALU.mult, op1=ALU.add),
                            [pko, ("x", s), ("comb", s)], [("x", s)])
                        m += 1

    b2f = flat_of(bias2)
    gu_ring = [(flat_of(slabs[i]), ("slab", i)) for i in range(NSLAB)] + [(b2f[:, 0:4096], ("hs", 0))]
    dn_ring = [(flat_of(qT), ("hs", 1)), (flat_of(zb)[:, 0:2048], ("hs", 2)), (gbc[:, 0, :].bitcast(BF16), ("hs", 3))]
    gu_i = [0]
    dn_i = [0]
    moe_seen = set()

    def load_moe(src_ap, down):
        if down:
            flat, key = dn_ring[dn_i[0] % len(dn_ring)]
            dn_i[0] += 1
            view = flat.rearrange("p (j d) -> p j d", j=2)
        else:
            flat, key = gu_ring[gu_i[0] % len(gu_ring)]
            gu_i[0] += 1
            view = flat.rearrange("p (k f) -> p k f", k=8)
        rd = []
        if key not in moe_seen:
            moe_seen.add(key)
            rd = ["ffn_go"]
        P.op("pool", lambda h: h.dma_start(out=view, in_=src_ap), reads=rd, writes=[key], dma=key)
        return view, key

    R_SEL, R_POSM, R_FF, R_FLI = 6144, 6272, 6400, 6448
    MOE_CHUNKS = [(0, 512)] + [(512 + 256 * i, 256) for i in range(6)]
    NFLAG = len(MOE_CHUNKS) - 1

    def moe_route_post():
        combf = comb[:].rearrange("p s e -> p (s e)")
        self_ = A(R_SEL, 128)
        selv = self_.rearrange("p (s e) -> p s e", s=NSUB)
        posm = A(R_POSM, 128)
        ff = A(R_FF, NFLAG * 8)
        fli = arena[:, R_FLI:R_FLI + NFLAG * 8].bitcast(I32)
        dve(lambda h: h.tensor_scalar(out=self_, in0=combf, scalar1=0.0, scalar2=None, op0=ALU.is_gt), [("comb", s) for s in range(NSUB)], ["sel"])
        for s in range(NSUB):
            mm(ps[4][:, s * 8:(s + 1) * 8], ustrict[:], selv[:, s, :], True, s == 0, ["sel"], [("ps", 4)])
            for s2 in range(s):
                mm(ps[4][:, s * 8:(s + 1) * 8], ones_f[:], selv[:, s2, :], False, s2 == s - 1, ["sel"], [("ps", 4)])
        for s in range(NSUB):
            mm(ps[5][:, 0:NE], ones_f[:], selv[:, s, :], s == 0, s == NSUB - 1, ["sel"], [("ps", 5)])
        dve(lambda h: h.scalar_tensor_tensor(out=posm, in0=ps[4][:, 0:128], scalar=1.0, in1=self_, op0=ALU.add, op1=ALU.mult), [("ps", 4), "sel"], ["posm"])
        dve(lambda h: h.tensor_scalar(out=posm, in0=posm, scalar1=-1.0, scalar2=1.0, op0=ALU.mult, op1=ALU.add), ["posm"], ["posm"])
        for q in range(1, NFLAG + 1):
            dve(lambda h, q=q: h.tensor_scalar(out=ff[:, (q - 1) * 8:q * 8], in0=ps[5][:, 0:NE], scalar1=float(MOE_CHUNKS[q][0]) - 0.5, scalar2=None, op0=ALU.is_gt),
                [("ps", 5)], ["ff"])
        dve(lambda h: h.tensor_copy(out=fli, in_=ff), ["ff"], ["fli"])

    def moe_chunk(l, e, slot0, NS):
        NU = NS // 128
        wguv = moe_wgu[l // 2, e].rearrange("(k p) f -> p k f", p=128)
        wdv = moe_wd[l // 2, e].rearrange("(j p) d -> p j d", p=128)
        xnb = kvact[:, 0:16384].rearrange("p (s d) -> p s d", s=16)
        posm = A(R_POSM, 128).rearrange("p (s e) -> p s e", s=NSUB)
        hg = hT
        modcol = modcols[l % 2]
        ABcol = ABcols[l % 2]
        ybf = flat_of(hT).rearrange("p (u d) -> p u d", u=4)
        hgkeys = [("hg", c) for c in range(8)]
        sgb = [A(0, 512), A(512, 512)]
        Gb = [AB(1024, 512), AB(1280, 512)]
        GTb = [AB(1536, 512), AB(1792, 512)]
        yacc = A(2048, 4096).rearrange("p (u d) -> p u d", u=4)
        acts = flat_of(ycat)[:, 0:2048].rearrange("p (b j t) -> p b j t", b=2, j=2)
        gi = [0]

        def build_G(s):
            b = gi[0] % 2
            gi[0] += 1
            G = Gb[b]
            dve(lambda h: h.tensor_scalar(out=G[:, 0:NS], in0=iota_f[:, 0:NS], scalar1=posm[:, s, e:e + 1], scalar2=float(-slot0), op0=ALU.add, op1=ALU.is_equal),
                ["posm"], [("G", b)])
            return G, ("G", b)

        for pas in range(2):
            for s in range(NSUB):
                G, gk = build_G(s)
                for cc in range(4):
                    c = pas * 4 + cc
                    mm(ps[4 + cc][:, 0:NS], xnb[:, s, c * 128:(c + 1) * 128], G[:, 0:NS], s == 0, s == NSUB - 1, [gk, ("xnb", s)], [("ps", 4 + cc)])
            for cc in range(4):
                c = pas * 4 + cc
                actop(lambda h, cc=cc, c=c: h.activation(out=hg[:, c, 0:NS], in_=ps[4 + cc][:, 0:NS], func=AF.Identity,
                                                       scale=ABcol[:, 1, c:c + 1], bias=modcol[:, 2, c:c + 1]),
                      [("ps", 4 + cc), ("ABcol", l % 2), ("modcol", l % 2)], [("hg", c)])
        n = 0
        mc = [0]

        def down_step(st, ab, sdw, kdw):
            for u in range(NU):
                for half in range(2):
                    m = mc[0]
                    po, pko = ps[4 + m % 4], ("ps", 4 + m % 4)
                    for jj in range(2):
                        mm(po[:, :], acts[:, ab, jj, u * 128:(u + 1) * 128], sdw[:, jj, half * 512:(half + 1) * 512], jj == 0, jj == 1,
                           [kdw, ("act", ab, jj)], [pko])
                    ya = yacc[:, u, half * 512:(half + 1) * 512]
                    if st == 0:
                        dve(lambda h, po=po, ya=ya: h.tensor_copy(out=ya, in_=po[:, :]), [pko], [("yacc", u, half)])
                    else:
                        dve(lambda h, po=po, ya=ya: h.tensor_tensor(out=ya, in0=po[:, :], in1=ya, op=ALU.add), [pko, ("yacc", u, half)], [("yacc", u, half)])
                    mc[0] += 1

        pend = None
        for st in range(FF_EXP // 256):
            j0 = 2 * st
            sgu, kgu = load_moe(wguv[:, :, st * 512:(st + 1) * 512], False)
            sgw, kgw = sgu[:, :, 0:256], kgu
            suw, kuw = sgu[:, :, 256:512], kgu
            sdw, kdw = load_moe(wdv[:, j0:j0 + 2, :], True)
            ab = st % 2
            for jj in range(2):
                pg, pkg = ps[n % 2], ("ps", n % 2)
                pu, pku = ps[2 + n % 2], ("ps", 2 + n % 2)
                sg = sgb[n % 2]
                for k in range(8):
                    mm(pg[:, 0:NS], sgw[:, k, jj * 128:(jj + 1) * 128], hg[:, k, 0:NS], k == 0, k == 7, [kgw] + hgkeys, [pkg])
                for k in range(8):
                    mm(pu[:, 0:NS], suw[:, k, jj * 128:(jj + 1) * 128], hg[:, k, 0:NS], k == 0, k == 7, [kuw] + hgkeys, [pku])
                actop(lambda h, sg=sg, pg=pg: h.activation(out=sg[:, 0:NS], in_=pg[:, 0:NS], func=AF.Silu), [pkg], [("sg", n % 2)])
                dve(lambda h, sg=sg, pu=pu, ab=ab, jj=jj: h.tensor_tensor(out=acts[:, ab, jj, 0:NS], in0=pu[:, 0:NS], in1=sg[:, 0:NS], op=ALU.mult),
                    [pku, ("sg", n % 2)], [("act", ab, jj)])
                n += 1
            if pend is not None:
                down_step(*pend)
            pend = (st, ab, sdw, kdw)
        down_step(*pend)
        m = mc[0]
        for u in range(NU):
            dve(lambda h, u=u: h.tensor_tensor(out=ybf[:, u, :], in0=yacc[:, u, :], in1=gbc[:, 1, :], op=ALU.mult),
                [("yacc", u, 0), ("yacc", u, 1), ("gbc", 1)], hgkeys)
        Gs = {}

        def ensure_G(s):
            if s < NSUB and s not in Gs:
                Gs[s] = build_G(s)

        def emit_T(s):
            G, gk = Gs[s]
            pt, pkt = ps[s % 2], ("ps", s % 2)
            for u in range(NU):
                mm(pt[:, u * 128:(u + 1) * 128], G[:, u * 128:(u + 1) * 128], ident_b[:], True, True, [gk], [pkt])
            GT = GTb[s % 2]
            actop(lambda h: h.activation(out=GT[:, 0:NS], in_=pt[:, 0:NS], func=AF.Identity), [pkt], [("GT", s % 2)])

        ensure_G(0)
        ensure_G(1)
        emit_T(0)
        for s in range(NSUB):
            if s + 1 < NSUB:
                emit_T(s + 1)
            ensure_G(s + 2)
            GT = GTb[s % 2]
            for half in range(2):
                po, pko = ps[4 + m % 4], ("ps", 4 + m % 4)
                for u in range(NU):
                    mm(po[:, :], GT[:, u * 128:(u + 1) * 128], ybf[:, u, half * 512:(half + 1) * 512], u == 0, u == NU - 1, [("GT", s % 2)] + hgkeys, [pko])
                dve(lambda h, po=po, half=half, s=s: h.scalar_tensor_tensor(out=x_tok[:, s, half * 512:(half + 1) * 512], in0=po[:, :], scalar=comb[:, s, e:e + 1],
                                                                          in1=x_tok[:, s, half * 512:(half + 1) * 512], op0=ALU.mult, op1=ALU.add),
                    [pko, ("x", s), ("comb", s)], [("x", s)])
                m += 1

    def moe_layer(l):
        dve(lambda h: h.memset(small[:, 80:81], 0.0), [], ["ffn_go"])
        moe_seen.clear()
        for t in range(NT):
            norm_tile(t, 1, True)
        P.barrier()
        moe_route_post()
        fli = arena[:, R_FLI:R_FLI + NFLAG * 8].bitcast(I32)
        for e in range(NE):
            moe_chunk(l, e, MOE_CHUNKS[0][0], MOE_CHUNKS[0][1])
        for e in range(NE):
            for q, (slot0, nsl) in enumerate(MOE_CHUNKS):
                if q > 0:
                    P.begin_region(fli[0:1, (q - 1) * 8 + e:(q - 1) * 8 + e + 1])
                    moe_chunk(l, e, slot0, nsl)
            for q in range(1, len(MOE_CHUNKS)):
                P.end_region()
        P.ops.append(("barrier_all",))

    def ffn_layer(l):
        moe = (l % 2 == 1)
        if moe and SPARSE_MOE:
            moe_layer(l)
            return
        h2all = kvact[:, 0:16384].rearrange("p (c t) -> p c t", c=8)
        dve(lambda h: h.memset(small[:, 80:81], 0.0), [], ["ffn_go"])
        for t in range(NT):
            norm_tile(t, 1, moe, dst=h2all)
        P.barrier()
        if moe:
            wts = [(moe_wg[l // 2, e], moe_wu[l // 2, e], moe_wd[l // 2, e], e) for e in range(NE)]
            ffn_experts(wts, FF_EXP // 128)
        else:
            hook = None
            if l + 1 in layers:
                todo = list(range(10))

                def hook():
                    for _ in range(2):
                        if todo:
                            mod_slab(l + 1, todo.pop(0))
            ffn_experts([(ffn_wg[l // 2], ffn_wu[l // 2], ffn_wg[l // 2] if False else ffn_wd[l // 2], None)], FF_DENSE // 128, hook)
            if l + 1 in layers:
                while todo:
                    mod_slab(l + 1, todo.pop(0))
                PRE_MOD.add(l + 1)
        P.barrier()

    PRE_MOD = set()
    for l in layers:
        LCUR[0] = l
        layer_consts(l)
        dve(lambda h: h.memset(zb[:, :, 0:30], 0.0), [], [("zb", c) for c in range(4)])
        if l in PRE_MOD:
            for sl in range(10, 12):
                mod_slab(l, sl)
            mod_finish(l)
            later = []
        else:
            for sl in range(4):
                mod_slab(l, sl)
            mod_finish(l, parts=(0,))
            later = list(range(4, 12))
        dve(lambda h: h.tensor_scalar(out=small[:, 64:65], in0=qkg[:, 0:1], scalar1=0.125, scalar2=None, op0=ALU.mult), ["qkg"], ["qkg8"])
        dve(lambda h: h.tensor_scalar(out=small[:, 65:66], in0=qkg[:, 1:2], scalar1=1.0, scalar2=None, op0=ALU.mult), ["qkg"], ["qkg8"])
        P.barrier()
        for t in range(NT):
            norm_tile(t, 0, False)
            if debug and "hT" in dbg_out:
                P.op("pool", lambda h, t=t: h.dma_start(out=dbg_out["hT"][t], in_=hT[:]), reads=[("hT", i) for i in range(4)], writes=[("dbg", 0)], dma=("dbg", 0))
            proj_tile(l, t)
            for _ in range(4):
                if later:
                    mod_slab(l, later.pop(0))
            P.barrier()
            conv_tile(l, t)
            P.barrier()
            if later and later[0] < 6:
                raise AssertionError("g1 slabs must precede the out-projection")
            attn_tile(l, t)
            for _ in range(4):
                if later:
                    mod_slab(l, later.pop(0))
            if later == [] and t == 0 and l not in PRE_MOD:
                mod_finish(l, parts=(1,))
            if debug and "ycat" in dbg_out:
                P.op("pool", lambda h, t=t: h.dma_start(out=dbg_out["ycat"][t], in_=ycat[:]), reads=[("ycat", i) for i in range(8)], writes=[("dbg", 1)], dma=("dbg", 1))
            outproj_tile(l, t)
        P.barrier()
        if debug and "xmid" in dbg_out:
            P.op("sp", lambda h: h.dma_start(out=dbg_out["xmid"].rearrange("(s p) d -> p s d", p=128), in_=x_tok[:]), reads=[("x", i) for i in range(16)], writes=[("dbg", 2)], dma=("dbg", 2))
        ffn_layer(l)

    yv = y_out.rearrange("(s p) d -> p s d", p=128)
    for s4 in range(4):
        P.op("sp", lambda h, s4=s4: h.dma_start(out=yv[:, 4 * s4:4 * s4 + 4, :], in_=x_tok[:, 4 * s4:4 * s4 + 4, :]),
             reads=[("x", 4 * s4 + i) for i in range(4)], writes=[("y", s4)], dma=("xio", s4))
    P.op("sp", None, reads=[("y", s4) for s4 in range(4)] + [("dbg", i) for i in range(3)], writes=[])

    P.emit(nc, stack)
    stack.close()
    return nc


def _bias2_index():
    q = np.arange(128)[:, None]
    j = np.arange(640)[None, :]
    rel = j - 512 - q
    idx = np.clip(rel, -128, 128) + 128
    valid = np.where(q < 64, j < 576, j >= 64)
    return idx, valid


def _interleave_gate_up(g, u):
    n, e, d, ff = g.shape
    st = ff // 256
    out = np.empty((n, e, d, st, 2, 256), np.float32)
    out[:, :, :, :, 0, :] = g.reshape(n, e, d, st, 256)
    out[:, :, :, :, 1, :] = u.reshape(n, e, d, st, 256)
    return out.reshape(n, e, d, 2 * ff)


def _host_layout(inp):
    f = np.float32
    idx, valid = _bias2_index()
    rel_bias = np.asarray(inp["rel_bias"], f)
    gathered = rel_bias[:, :, idx]
    bias2 = np.where(valid[None, None], gathered, f(MASK_NEG)).astype(f)
    bias2 = np.ascontiguousarray(bias2.transpose(0, 2, 1, 3))

    def col8(v):
        return np.ascontiguousarray(np.asarray(v, f).reshape(DEPTH, 8, 128).transpose(0, 2, 1))

    def col4(v):
        return np.asarray(v, f).reshape(DEPTH, 4, 128).transpose(0, 2, 1)

    convw = np.ascontiguousarray(np.asarray(inp["conv_w"], f)[:, :, 0, :].reshape(DEPTH, 31, 4, 128).transpose(0, 3, 2, 1))
    convp = np.ascontiguousarray(np.stack([col4(inp["conv_b"]), col4(inp["conv_ln_g"]), col4(inp["conv_ln_b"])], axis=2))
    qg = np.asarray(inp["q_norm_g"], f)
    kg = np.asarray(inp["k_norm_g"], f)
    qkg = np.ascontiguousarray(np.stack([np.tile(qg, (1, 2)), np.tile(kg, (1, 2))], axis=2))
    wr = np.ascontiguousarray(np.asarray(inp["moe_w_router"], f).reshape(1, 8, 128, NE).transpose(0, 2, 1, 3))
    br = np.ascontiguousarray(np.broadcast_to(np.asarray(inp["moe_b_router"], f)[:, None, :], (1, 128, NE)))
    shared = {
        "w_ada": np.ascontiguousarray(inp["w_ada"], f), "b_ada": np.ascontiguousarray(inp["b_ada"], f),
        "gmix": col8(inp["norm_mix_g"]), "gffn": col8(inp["norm_ffn_g"]),
        "w_in": np.ascontiguousarray(inp["w_in"], f), "w_out": np.ascontiguousarray(inp["w_out"], f),
        "convw": convw, "convp": convp, "qkg": qkg, "bias2": bias2,
        "ffn_wg": np.ascontiguousarray(inp["ffn_w_gate"], f), "ffn_wu": np.ascontiguousarray(inp["ffn_w_up"], f),
        "ffn_wd": np.ascontiguousarray(inp["ffn_w_down"], f),
        "wr": wr, "br": br,
        "moe_wgu": _interleave_gate_up(np.asarray(inp["moe_w_gate"], f), np.asarray(inp["moe_w_up"], f)),
        "moe_wd": np.ascontiguousarray(inp["moe_w_down"], f),
        "ident": np.eye(128, dtype=f),
        "ustrict": np.triu(np.ones((128, 128), f), 1),
        "iota": np.ascontiguousarray(np.broadcast_to(np.arange(512, dtype=f)[None, :], (128, 512))),
    }
    c = np.asarray(inp["c"], f)
    cT = [np.ascontiguousarray(c[b].reshape(8, 128).T) for b in range(8)]
    return shared, cT


LAUNCH_GROUPS = [[0, 1]]
_PROG_CACHE = {}


def kernel(**inputs):
    shared, cT = _host_layout(inputs)
    x = np.ascontiguousarray(np.asarray(inputs["x"], np.float32))
    cur = [x[b] for b in range(8)]
    for grp in LAUNCH_GROUPS:
        key = tuple(grp)
        if key not in _PROG_CACHE:
            _PROG_CACHE[key] = build_program(grp)
        nc = _PROG_CACHE[key]
        in_maps = []
        drop = set()
        if 1 not in grp:
            drop |= {"moe_wgu", "moe_wd"}
        if 0 not in grp:
            drop |= {"ffn_wg", "ffn_wu", "ffn_wd"}
        for b in range(8):
            m = {k: v for k, v in shared.items() if k not in drop}
            m["x"] = cur[b]
            m["cT"] = cT[b]
            in_maps.append(m)
        res = run_bass_kernel_spmd(nc, in_maps, core_ids=list(range(8)))
        cur = [np.asarray(res.results[b]["y"], np.float32) for b in range(8)]
    return np.stack(cur, axis=0)
```
